# Optimizing a Trainium2 kernel written in Bass

```python
import jax, jax.numpy as jnp
from jax import lax
import numpy as np

D_MODEL = 1024
BATCH = 8
SEQ = 2048
DEPTH = 4

MIX_WIDTH = D_MODEL
RET_WIDTH = MIX_WIDTH // 2
GLA_WIDTH = MIX_WIDTH - RET_WIDTH
RET_HEADS = 4
RET_HEAD_DIM = RET_WIDTH // RET_HEADS
GLA_HEADS = 4
GLA_KEY_WIDTH = GLA_WIDTH // 2
GLA_DK = GLA_KEY_WIDTH // GLA_HEADS
GLA_DV = GLA_WIDTH // GLA_HEADS
GLA_GATE_RANK = 16
GLA_GATE_TAU = 16.0
CHUNK = 64
ROPE_BASE = 10000.0
D_FF_DENSE = 2816
N_EXPERTS = 8
TOP_K = 2
D_FF_EXPERT = 3584
EPS = 1e-6
N_DENSE = (DEPTH + 1) // 2
N_MOE = DEPTH // 2
IN_SPLITS = (RET_WIDTH, RET_WIDTH, RET_WIDTH, RET_WIDTH,
             GLA_KEY_WIDTH, GLA_KEY_WIDTH, GLA_WIDTH, GLA_WIDTH, GLA_GATE_RANK)
IN_COLS = 4 * RET_WIDTH + 2 * GLA_KEY_WIDTH + 2 * GLA_WIDTH + GLA_GATE_RANK

kernel_name = "hymba_retnet_gla_moe_adaln"


def rms_norm(x, g):
    xf = x.astype(jnp.float32)
    y = xf * lax.rsqrt(jnp.mean(xf * xf, axis=-1, keepdims=True) + EPS)
    return (y * g.astype(jnp.float32)).astype(x.dtype)


def head_norm(o):
    return o * lax.rsqrt(jnp.mean(o * o, axis=-1, keepdims=True) + EPS)


def rotary(x, pos):
    half = x.shape[-1] // 2
    inv = ROPE_BASE ** (-jnp.arange(half, dtype=jnp.float32) / half)
    ang = pos.astype(jnp.float32)[:, None] * inv[None, :]
    cos = jnp.cos(ang)[None, :, None, :]
    sin = jnp.sin(ang)[None, :, None, :]
    x1, x2 = x[..., :half], x[..., half:]
    return jnp.concatenate([x1 * cos - x2 * sin, x1 * sin + x2 * cos], axis=-1)


def to_chunks(t):
    b, s, h, d = t.shape
    return t.reshape(b, s // CHUNK, CHUNK, h, d).transpose(0, 3, 1, 2, 4)


def from_chunks(t):
    b, h, n, c, d = t.shape
    return t.transpose(0, 2, 3, 1, 4).reshape(b, n * c, h, d)


def retention_chunkwise(q, k, v, log_gamma):
    c = q.shape[3]
    idx = jnp.arange(c, dtype=jnp.float32)
    diff = idx[:, None] - idx[None, :]
    causal = diff >= 0
    decay_mat = jnp.where(causal[None],
                          jnp.exp(log_gamma[:, None, None] * jnp.where(causal, diff, 0.0)[None]),
                          0.0)
    scores = jnp.einsum('bhncd,bhnsd->bhncs', q, k) * decay_mat[None, :, None]
    intra = jnp.einsum('bhncs,bhnse->bhnce', scores, v)
    q_in = q * jnp.exp(log_gamma[:, None] * (idx + 1.0))[None, :, None, :, None]
    k_st = k * jnp.exp(log_gamma[:, None] * (c - 1.0 - idx))[None, :, None, :, None]
    contrib = jnp.einsum('bhnsd,bhnse->nbhde', k_st, v)
    gamma_c = jnp.exp(log_gamma * c)[None, :, None, None]

    def step(state, inc):
        return gamma_c * state + inc, state

    init = jnp.zeros(contrib.shape[1:], jnp.float32)
    _, prev = lax.scan(step, init, contrib)
    inter = jnp.einsum('bhncd,nbhde->bhnce', q_in, prev)
    return intra + inter


def gla_chunked(q, k, v, log_a):
    c = q.shape[3]
    b = jnp.cumsum(log_a, axis=3)
    b_ref = b[:, :, :, c // 2 - 1:c // 2]
    q_i = q * jnp.exp(b - b_ref)
    k_i = k * jnp.exp(b_ref - b)
    causal = jnp.tril(jnp.ones((c, c), dtype=bool))
    scores = jnp.where(causal, jnp.einsum('bhncd,bhnsd->bhncs', q_i, k_i), 0.0)
    intra = jnp.einsum('bhncs,bhnse->bhnce', scores, v)
    b_last = b[:, :, :, -1:]
    q_inter = q * jnp.exp(b)
    k_state = k * jnp.exp(b_last - b)
    contrib = jnp.einsum('bhnsd,bhnse->nbhde', k_state, v)
    decay = jnp.exp(b_last[:, :, :, 0]).transpose(2, 0, 1, 3)

    def step(state, inp):
        d, inc = inp
        return d[..., None] * state + inc, state

    init = jnp.zeros(contrib.shape[1:], jnp.float32)
    _, prev = lax.scan(step, init, (decay, contrib))
    inter = jnp.einsum('bhncd,nbhde->bhnce', q_inter, prev)
    return intra + inter


def hybrid_mixer(h, w_in, w_gla_gate2, b_gla_gate, w_out):
    bsz, s, _ = h.shape
    pos = jnp.arange(s)
    f32 = jnp.float32
    proj = (h @ w_in).astype(f32)
    points, acc = [], 0
    for width in IN_SPLITS[:-1]:
        acc += width
        points.append(acc)
    rq, rk, rv, rg, gq, gk, gv, gr, ga = jnp.split(proj, points, axis=-1)

    rq = rotary(rq.reshape(bsz, s, RET_HEADS, RET_HEAD_DIM), pos)
    rk = rotary(rk.reshape(bsz, s, RET_HEADS, RET_HEAD_DIM), pos) * (RET_HEAD_DIM ** -0.5)
    rv = rv.reshape(bsz, s, RET_HEADS, RET_HEAD_DIM)
    log_gamma = jnp.log(1.0 - jnp.exp2(-5.0 - jnp.arange(RET_HEADS, dtype=f32)))
    ret = from_chunks(retention_chunkwise(to_chunks(rq), to_chunks(rk), to_chunks(rv), log_gamma))
    ret = head_norm(ret).reshape(bsz, s, RET_WIDTH) * jax.nn.silu(rg)

    gq = gq.reshape(bsz, s, GLA_HEADS, GLA_DK) * (GLA_DK ** -0.5)
    gk = gk.reshape(bsz, s, GLA_HEADS, GLA_DK)
    gv = gv.reshape(bsz, s, GLA_HEADS, GLA_DV)
    gate_logits = ga @ w_gla_gate2.astype(f32) + b_gla_gate.astype(f32)
    log_a = (jax.nn.log_sigmoid(gate_logits) / GLA_GATE_TAU).reshape(bsz, s, GLA_HEADS, GLA_DK)
    gla = from_chunks(gla_chunked(to_chunks(gq), to_chunks(gk), to_chunks(gv), to_chunks(log_a)))
    gla = head_norm(gla).reshape(bsz, s, GLA_WIDTH) * jax.nn.silu(gr)

    merged = jnp.concatenate([ret, gla], axis=-1).astype(h.dtype)
    return merged @ w_out


def swiglu(h, w_gate, w_up, w_down):
    return (jax.nn.silu(h @ w_gate) * (h @ w_up)) @ w_down


def moe_swiglu(h, w_router, w_gate, w_up, w_down):
    logits = (h @ w_router).astype(jnp.float32)
    top_vals, top_idx = lax.top_k(logits, TOP_K)
    top_w = jax.nn.softmax(top_vals, axis=-1)
    gates = jnp.sum(jax.nn.one_hot(top_idx, N_EXPERTS, dtype=jnp.float32) * top_w[..., None],
                    axis=-2)
    out = jnp.zeros_like(h)
    for e in range(N_EXPERTS):
        out = out + gates[..., e:e + 1].astype(h.dtype) * swiglu(h, w_gate[e], w_up[e], w_down[e])
    return out


def setup_inputs(seed: int = 0) -> dict:
    key = jax.random.key(seed)
    ks = jax.random.split(key, 24)
    f32 = jnp.float32
    nrm = lambda k, shape, scale: jax.random.normal(k, shape, f32) * scale
    return {
        "x": nrm(ks[0], (BATCH, SEQ, D_MODEL), 1.0),
        "c": nrm(ks[1], (BATCH, D_MODEL), 1.0),
        "ada_w": nrm(ks[2], (DEPTH, D_MODEL, 6 * D_MODEL), 0.5 * D_MODEL ** -0.5),
        "ada_b": nrm(ks[3], (DEPTH, 6 * D_MODEL), 0.01),
        "norm_mix_g": 1.0 + nrm(ks[4], (DEPTH, D_MODEL), 0.02),
        "norm_ffn_g": 1.0 + nrm(ks[5], (DEPTH, D_MODEL), 0.02),
        "w_in": nrm(ks[6], (DEPTH, D_MODEL, IN_COLS), D_MODEL ** -0.5),
        "w_gla_gate2": nrm(ks[7], (DEPTH, GLA_GATE_RANK, GLA_KEY_WIDTH), GLA_GATE_RANK ** -0.5),
        "b_gla_gate": nrm(ks[8], (DEPTH, GLA_KEY_WIDTH), 0.1),
        "w_out": nrm(ks[9], (DEPTH, MIX_WIDTH, D_MODEL), MIX_WIDTH ** -0.5),
        "dense_w_gate": nrm(ks[10], (N_DENSE, D_MODEL, D_FF_DENSE), D_MODEL ** -0.5),
        "dense_w_up": nrm(ks[11], (N_DENSE, D_MODEL, D_FF_DENSE), D_MODEL ** -0.5),
        "dense_w_down": nrm(ks[12], (N_DENSE, D_FF_DENSE, D_MODEL), D_FF_DENSE ** -0.5),
        "w_router": nrm(ks[13], (N_MOE, D_MODEL, N_EXPERTS), D_MODEL ** -0.5),
        "moe_w_gate": nrm(ks[14], (N_MOE, N_EXPERTS, D_MODEL, D_FF_EXPERT), D_MODEL ** -0.5),
        "moe_w_up": nrm(ks[15], (N_MOE, N_EXPERTS, D_MODEL, D_FF_EXPERT), D_MODEL ** -0.5),
        "moe_w_down": nrm(ks[16], (N_MOE, N_EXPERTS, D_FF_EXPERT, D_MODEL), D_FF_EXPERT ** -0.5),
        "final_g": 1.0 + nrm(ks[17], (D_MODEL,), 0.02),
    }


def reference(x, c, ada_w, ada_b, norm_mix_g, norm_ffn_g, w_in, w_gla_gate2, b_gla_gate,
              w_out, dense_w_gate, dense_w_up, dense_w_down, w_router, moe_w_gate,
              moe_w_up, moe_w_down, final_g):
    c_act = jax.nn.silu(c)
    for l in range(DEPTH):
        mod = c_act @ ada_w[l] + ada_b[l]
        shift_m, scale_m, gate_m, shift_f, scale_f, gate_f = jnp.split(mod, 6, axis=-1)
        h = rms_norm(x, norm_mix_g[l]) * (1.0 + scale_m[:, None]) + shift_m[:, None]
        x = x + gate_m[:, None] * hybrid_mixer(h, w_in[l], w_gla_gate2[l], b_gla_gate[l], w_out[l])
        h = rms_norm(x, norm_ffn_g[l]) * (1.0 + scale_f[:, None]) + shift_f[:, None]
        if l % 2 == 0:
            j = l // 2
            y = swiglu(h, dense_w_gate[j], dense_w_up[j], dense_w_down[j])
        else:
            j = l // 2
            y = moe_swiglu(h, w_router[j], moe_w_gate[j], moe_w_up[j], moe_w_down[j])
        x = x + gate_f[:, None] * y
    return rms_norm(x, final_g)
```

```python
import math
import types
import numpy as np
import concourse.bass as bass
import concourse.mybir as mybir
from concourse.bass_utils import run_bass_kernel_spmd

F32 = mybir.dt.float32
BF16 = mybir.dt.bfloat16
AF = mybir.ActivationFunctionType
ALU = mybir.AluOpType
AX = mybir.AxisListType

ENGS = ("pe", "act", "dve", "pool", "sp")
EPOCH = 8000

D = 1024
S = 2048
NT = 16
EPS = 1e-6
WIN_COLS = 4624
PIPE_W = 3


class T:
    __slots__ = ("name", "writer", "readers", "dsem", "dcount", "excl")

    def __init__(self, name, excl=False):
        self.name = name
        self.excl = excl
        self.writer = None
        self.readers = []
        self.dsem = None
        self.dcount = 0


class Op:
    __slots__ = ("eng", "fn", "deps", "is_dma", "dma_tile", "dma_val", "sig", "sigidx", "waits", "id", "pos")


def _freeze(fn):
    if fn.__closure__ is None:
        return fn
    cells = []
    for c in fn.__closure__:
        try:
            cells.append(types.CellType(c.cell_contents))
        except ValueError:
            cells.append(c)
    return types.FunctionType(fn.__code__, fn.__globals__, fn.__name__, fn.__defaults__, tuple(cells))


class Prog:
    def __init__(self, nc):
        self.nc = nc
        self.ops = []
        self.eng_ops = {e: [] for e in ENGS}

    def op(self, eng, fn, reads=(), writes=(), dma_out=None):
        o = Op()
        o.id = len(self.ops)
        o.eng = eng
        o.fn = _freeze(fn)
        o.is_dma = False
        o.dma_tile = None
        o.dma_val = 0
        o.sig = False
        o.sigidx = -1
        o.waits = []
        deps = set()
        for t in reads:
            if t.writer is not None:
                deps.add(t.writer)
            if t.excl:
                for r in t.readers:
                    if self.ops[r].eng != eng:
                        deps.add(r)
        for t in writes:
            if t.writer is not None:
                deps.add(t.writer)
            for r in t.readers:
                deps.add(r)
        o.deps = sorted(deps)
        if dma_out is not None:
            o.is_dma = True
            o.dma_tile = dma_out
            if dma_out.dsem is None:
                dma_out.dsem = self.nc.alloc_semaphore("d_" + dma_out.name)
            dma_out.dcount += 16
            o.dma_val = dma_out.dcount
        for t in reads:
            t.readers.append(o.id)
        for t in writes:
            t.writer = o.id
            t.readers = []
        self.ops.append(o)
        self.eng_ops[eng].append(o)
        return o

    def finalize(self):
        nc = self.nc
        ops = self.ops
        for e in ENGS:
            for i, o in enumerate(self.eng_ops[e]):
                o.pos = i
        eidx = {e: i for i, e in enumerate(ENGS)}
        NE = len(ENGS)
        clock = {e: [-1] * NE for e in ENGS}
        opclock = [None] * len(ops)
        dma_known = {e: {} for e in ENGS}
        for o in ops:
            ck = clock[o.eng]
            dk = dma_known[o.eng]
            best = {}
            dmas = {}
            for d in o.deps:
                p = ops[d]
                if p.is_dma:
                    key = id(p.dma_tile)
                    if key not in dmas or dmas[key].dma_val < p.dma_val:
                        dmas[key] = p
                else:
                    if p.eng == "pe" and o.eng == "pe" and not o.is_dma:
                        continue
                    if p.eng not in best or best[p.eng].pos < p.pos:
                        best[p.eng] = p
            for key, p in dmas.items():
                if dk.get(key, 0) >= p.dma_val:
                    continue
                dk[key] = p.dma_val
                o.waits.append(p)
                pc = opclock[p.id]
                for i in range(NE):
                    if pc[i] > ck[i]:
                        ck[i] = pc[i]
            for e2, p in best.items():
                pi = eidx[e2]
                if ck[pi] >= p.pos:
                    continue
                o.waits.append(p)
                p.sig = True
                pc = opclock[p.id]
                for i in range(NE):
                    if pc[i] > ck[i]:
                        ck[i] = pc[i]
                if ck[pi] < p.pos:
                    ck[pi] = p.pos
            snap = list(ck)
            if not o.is_dma:
                snap[eidx[o.eng]] = max(snap[eidx[o.eng]], o.pos)
            opclock[o.id] = snap
        nsig = {e: 0 for e in ENGS}
        for e in ENGS:
            for o in self.eng_ops[e]:
                if o.sig and not o.is_dma:
                    o.sigidx = nsig[e]
                    nsig[e] += 1
        self.esems = {}
        for e in ENGS:
            n = (nsig[e] + EPOCH - 1) // EPOCH
            self.esems[e] = [nc.alloc_semaphore(f"s_{e}{i}") for i in range(n)]
        for o in ops:
            w = []
            for p in o.waits:
                if p.is_dma:
                    w.append((p.dma_tile.dsem, p.dma_val))
                else:
                    w.append((self.esems[p.eng][p.sigidx // EPOCH], p.sigidx % EPOCH + 1))
            o.waits = w

    def emit_engine(self, e, eng):
        for o in self.eng_ops[e]:
            for (sem, val) in o.waits:
                eng.wait_ge(sem, val)
            ins = o.fn(eng)
            if o.is_dma:
                ins.then_inc(o.dma_tile.dsem, 16)
            elif o.sigidx >= 0:
                ins.then_inc(self.esems[e][o.sigidx // EPOCH], 1)

    def run(self):
        self.finalize()
        with self.nc.Block() as block:
            @block.tensor
            def _(eng):
                self.emit_engine("pe", eng)

            @block.scalar
            def _(eng):
                self.emit_engine("act", eng)

            @block.vector
            def _(eng):
                self.emit_engine("dve", eng)

            @block.gpsimd
            def _(eng):
                self.emit_engine("pool", eng)

            @block.sync
            def _(eng):
                self.emit_engine("sp", eng)


def build(depth=4, do_mixer=True, do_ffn=True, layers=None, mix_stage=99):
    nc = bass.Bass("TRN2", target_bir_lowering=False)
    P = Prog(nc)

    def din(name, shape):
        return nc.dram_tensor(name, list(shape), F32, kind="ExternalInput").ap()

    x_d = din("x", [S, D])
    c_d = din("c", [128, 8])
    adaw_d = din("ada_w", [4, D, 6 * D])
    adab_d = din("ada_b", [4, 6 * D])
    gmix_d = din("gmix", [4, D])
    gffn_d = din("gffn", [4, D])
    gfin_d = din("gfin", [1, D])
    win_d = din("w_in", [4, D, WIN_COLS])
    w2_d = din("w2", [4, 16, 256])
    b2_d = din("b2", [4, 256])
    wout_d = din("w_out", [4, D, D])
    dwg_d = din("dwg", [2, D, 2816])
    dwu_d = din("dwu", [2, D, 2816])
    dwd_d = din("dwd", [2, 2816, D])
    wr_d = din("wr", [2, D, 8])
    mwg_d = din("mwg", [2, 8, D, 3584])
    mwu_d = din("mwu", [2, 8, D, 3584])
    mwd_d = din("mwd", [2, 8, 3584, D])
    idf_d = din("idf", [128, 128])
    tri_d = din("tri", [128, 128])
    cos_d = din("cos", [128, S])
    sin_d = din("sin", [128, S])
    maskR_d = din("maskR", [128, 512])
    kdec_d = din("kdec", [128, 512])
    gqtab_d = din("gqtab", [128, 512])
    maskG_d = din("maskG", [128, 512])
    rmask_d = din("rmask", [128, 2])
    out_d = nc.dram_tensor("out", [S, D], F32, kind="ExternalOutput").ap()

    def sb(name, shape, dt):
        return nc.alloc_sbuf_tensor("s_" + name, list(shape), dt)

    xt = sb("xt", [128, NT, D], F32)
    Tx = [T(f"x{i}") for i in range(NT)]
    hT = sb("hT", [128, 8, S], BF16)
    Th = [T(f"h{i}") for i in range(4)]
    NR = 4
    ring = [sb(f"ring{i}", [128, 4096], BF16) for i in range(NR)]
    Tring = [T(f"ring{i}") for i in range(NR)]
    ring_i = [0]

    def rload(src, a, b):
        i = ring_i[0] % NR
        ring_i[0] += 1
        view = ring[i][:, 0:a * b].rearrange("p (a b) -> p a b", b=b)
        P.op("pool", lambda e: e.dma_start(out=view, in_=src), writes=[Tring[i]], dma_out=Tring[i])
        return view, Tring[i]

    banks = [nc.alloc_psum_tensor(f"ps{i}", [128, 512], F32) for i in range(8)]
    TB = [T(f"ps{i}", excl=True) for i in range(8)]
    bank_i = [0]

    def nb():
        i = bank_i[0] % 8
        bank_i[0] += 1
        return i

    A4 = [sb(f"A4_{i}", [128, 4, 512], BF16) for i in range(6)]
    TA4 = [T(f"A4_{i}") for i in range(6)]
    F1 = [sb(f"F1_{i}", [128, 1024], F32) for i in range(3)]
    TF1 = [T(f"F1_{i}") for i in range(3)]
    F5 = [sb(f"F5_{i}", [128, 512], F32) for i in range(4)]
    TF5 = [T(f"F5_{i}") for i in range(4)]
    gate_bc = sb("gate_bc", [128, D], F32)
    Tgate = T("gate_bc")
    gaT = F5[3]
    TgaT = TF5[3]
    junk3 = A4[5][:, 0:2, :]
    Tjunk = TA4[5]
    adab_bc = [F5[2], F5[3]]
    Tadab = [TF5[2], TF5[3]]
    ss = sb("ss", [128, NT], F32)
    Tss = T("ss")
    rstd = sb("rstd", [128, NT], F32)
    Trstd = T("rstd")
    cosT = sb("cosT", [128, S], BF16)
    sinT = sb("sinT", [128, S], BF16)
    Ttab = T("tab")
    maskR = sb("maskR", [128, 512], BF16)
    kdec = sb("kdec", [128, 512], BF16)
    gqtab = sb("gqtab", [128, 512], BF16)
    maskG = sb("maskG", [128, 512], BF16)
    idf = sb("idf", [128, 128], F32)
    idb = sb("idb", [128, 128], BF16)
    tri = sb("tri", [128, 128], F32)
    rmask = sb("rmask", [128, 2], F32)
    onesb = sb("onesb", [128, 128], BF16)
    ones_row = sb("ones_row", [1, 128], F32)
    Tconst = T("const")
    Tidb = T("idb")
    ccol = sb("ccol", [128, 8], F32)
    Cbc = sb("Cbc", [128, 8, 128], BF16)
    TC = T("C")
    w2s = sb("w2s", [16, 256], F32)
    b2s = sb("b2s", [1, 256], F32)
    Tw2 = T("w2")
    wrs = sb("wrs", [128, 8, 8], BF16)
    Twr = T("wr")
    lg = sb("lg", [128, NT, 8], F32)
    gates = sb("gates", [128, NT, 8], F32)
    m12 = sb("m12", [128, 4, NT], F32)
    Tlg = T("lg")
    Tgates = T("gates")
    PTr = sb("PTr", [128, 512], BF16); TPTr = T("PTr")
    PTg = PTr; TPTg = TPTr
    kst = sb("kst", [128, 512], BF16); Tkst = T("kst")
    sq = sb("sq", [128, 512], BF16); Tsq = T("sq")
    Sr = sb("Sr", [128, 512], F32); TSr = T("Sr")
    Srb = sb("Srb", [128, 512], BF16); TSrb = T("Srb")
    Sg = sb("Sg", [128, 2, 128], F32); TSg = T("Sg")
    Sgb = sb("Sgb", [128, 2, 128], BF16); TSgb = T("Sgb")
    esp = sb("esp", [128, 256], F32); Tesp = T("esp")
    E1 = sb("E1", [128, 256], F32); TE1 = T("E1")
    E2 = sb("E2", [128, 256], F32); TE2 = T("E2")
    lg2 = E2[:, 0:128].rearrange("p (i c) -> p i c", c=8)
    qi2 = sb("qi2", [128, 2, 2, 128], BF16); Tqi = T("qi")
    ki = sb("ki", [128, 2, 128], BF16); Tki = T("ki")
    kitm = kst[:, 0:256]; Tkitm = Tkst
    tmpc = E1[:].rearrange("p (a t) -> p a t", t=128); Ttmpc = TE1
    sm = sb("sm", [128, 16], F32); Tsm = T("sm")
    Tout = T("out_store")

    P.op("sp", lambda e: e.dma_start(out=idf[:], in_=idf_d), writes=[Tconst], dma_out=Tconst)
    P.op("sp", lambda e: e.dma_start(out=tri[:], in_=tri_d), writes=[Tconst], dma_out=Tconst)
    P.op("sp", lambda e: e.dma_start(out=rmask[:], in_=rmask_d), writes=[Tconst], dma_out=Tconst)
    P.op("pool", lambda e: e.dma_start(out=idb[:], in_=idf_d), writes=[Tidb], dma_out=Tidb)
    P.op("pool", lambda e: e.dma_start(out=cosT[:], in_=cos_d), writes=[Ttab], dma_out=Ttab)
    P.op("pool", lambda e: e.dma_start(out=sinT[:], in_=sin_d), writes=[Ttab], dma_out=Ttab)
    P.op("pool", lambda e: e.dma_start(out=maskR[:], in_=maskR_d), writes=[Ttab], dma_out=Ttab)
    P.op("pool", lambda e: e.dma_start(out=kdec[:], in_=kdec_d), writes=[Ttab], dma_out=Ttab)
    P.op("pool", lambda e: e.dma_start(out=gqtab[:], in_=gqtab_d), writes=[Ttab], dma_out=Ttab)
    P.op("pool", lambda e: e.dma_start(out=maskG[:], in_=maskG_d), writes=[Ttab], dma_out=Ttab)
    Tones = T("ones")
    P.op("dve", lambda e: e.memset(onesb[:], 1.0 / 128.0), writes=[Tones])
    P.op("dve", lambda e: e.memset(ones_row[:], 1.0), writes=[Tones])
    P.op("sp", lambda e: e.dma_start(out=ccol[:], in_=c_d), writes=[TC], dma_out=TC)
    for i in range(NT):
        P.op("sp", lambda e, i=i: e.dma_start(out=xt[:, i, :], in_=x_d[i * 128:(i + 1) * 128, :]), writes=[Tx[i]], dma_out=Tx[i])
    P.op("act", lambda e: e.activation(out=ccol[:], in_=ccol[:], func=AF.Silu), reads=[TC], writes=[TC])
    P.op("dve", lambda e: e.tensor_copy(out=Cbc[:], in_=ccol[:].unsqueeze(2).to_broadcast([128, 8, 128])), reads=[TC], writes=[TC])

    def mod_tile(l, vec, dst, Tdst, kind, gsrc=None):
        if kind == "scale":
            gb, Tgb = F1[2], TF1[2]
            P.op("sp", lambda e: e.dma_start(out=gb[:], in_=gsrc.to_broadcast([128, D])), writes=[Tgb], dma_out=Tgb)
        for half in range(2):
            c0 = vec * D + half * 512
            pan, Tp = rload(adaw_d[l][:, c0:c0 + 512].rearrange("(k p) c -> p k c", p=128), 8, 512)
            ab, Tab = adab_bc[half], Tadab[half]
            P.op("sp", lambda e, ab=ab, c0=c0: e.dma_start(out=ab[:], in_=adab_d[l:l + 1, c0:c0 + 512].to_broadcast([128, 512])),
                 writes=[Tab], dma_out=Tab)
            b = nb()
            for k in range(8):
                P.op("pe", lambda e, b=b, k=k, pan=pan: e.matmul(banks[b][:], Cbc[:, k, :], pan[:, k, :], start=(k == 0), stop=(k == 7)),
                     reads=[TC, Tp], writes=[TB[b]])
            dsl = dst[:, half * 512:(half + 1) * 512]
            P.op("dve", lambda e, b=b, dsl=dsl, ab=ab: e.tensor_tensor(out=dsl, in0=banks[b][:], in1=ab[:], op=ALU.add),
                 reads=[TB[b], Tab], writes=[Tdst])
            if kind == "scale":
                gsl = gb[:, half * 512:(half + 1) * 512]
                P.op("dve", lambda e, dsl=dsl, gsl=gsl: e.scalar_tensor_tensor(out=dsl, in0=dsl, scalar=1.0, in1=gsl, op0=ALU.add, op1=ALU.mult),
                     reads=[Tdst, Tgb], writes=[Tdst])

    def rms_stats():
        P.op("dve", lambda e: e.memset(ss[:], 0.0), writes=[Tss])
        for i in range(NT):
            P.op("act", lambda e, i=i: e.activation(out=junk3, in_=xt[:, i, :].rearrange("p (a b) -> p a b", b=512), func=AF.Square, accum_out=ss[:, i:i + 1]),
                 reads=[Tx[i]], writes=[Tjunk, Tss])
        P.op("act", lambda e: e.activation(out=rstd[:], in_=ss[:], func=AF.Ln, bias=EPS, scale=1.0 / D), reads=[Tss], writes=[Trstd])
        P.op("act", lambda e: e.activation(out=rstd[:], in_=rstd[:], func=AF.Exp, scale=-0.5), reads=[Trstd], writes=[Trstd])

    def norm_to_hT(l, which):
        gs, Tgs = F1[0], TF1[0]
        sh, Tsh = F1[1], TF1[1]
        gsrc = (gmix_d if which == 0 else gffn_d)[l:l + 1, :]
        rms_stats()
        mod_tile(l, 3 * which + 0, sh, Tsh, "plain")
        mod_tile(l, 3 * which + 1, gs, Tgs, "scale", gsrc)
        mod_tile(l, 3 * which + 2, gate_bc, Tgate, "plain")
        xnb = [(F1[2][:], TF1[2]), (A4[2][:].bitcast(F32).rearrange("p a b -> p (a b)"), TA4[2])]
        for i in range(NT):
            xn, Txn = xnb[i % 2]
            P.op("dve", lambda e, i=i: e.scalar_tensor_tensor(out=xn, in0=xt[:, i, :], scalar=rstd[:, i:i + 1], in1=gs[:],
                                                              op0=ALU.mult, op1=ALU.mult), reads=[Tx[i], Trstd, Tgs], writes=[Txn])
            P.op("dve", lambda e: e.tensor_tensor(out=xn, in0=xn, in1=sh[:], op=ALU.add), reads=[Txn, Tsh], writes=[Txn])
            for half in range(2):
                b = nb()
                for j in range(4):
                    k = half * 4 + j
                    P.op("pe", lambda e, b=b, j=j, k=k: e.transpose(out=banks[b][:, j * 128:(j + 1) * 128], in_=xn[:, k * 128:(k + 1) * 128], identity=idf[:]),
                         reads=[Txn, Tconst], writes=[TB[b]])
                P.op("act", lambda e, b=b, half=half, i=i: e.copy(out=hT[:, half * 4:half * 4 + 4, i * 128:(i + 1) * 128],
                                                                   in_=banks[b][:].rearrange("p (j t) -> p j t", t=128)),
                     reads=[TB[b]], writes=[Th[i // 4]])

    ffn_state = {"cnt": 0, "dcnt": 0, "pending": None}

    def ffn_down(args):
        actb, Tact, wd, Twd, nfc, tt, gate_col = args
        db = [4, 5, 6, 7]
        for ti in range(4):
            tile = tt * 4 + ti
            bs_ = []
            for dt in range(2):
                b = db[ffn_state["dcnt"] % 4]
                ffn_state["dcnt"] += 1
                bs_.append(b)
                for fc in range(nfc):
                    P.op("pe", lambda e, b=b, fc=fc, ti=ti, dt=dt: e.matmul(banks[b][:], actb[:, fc, ti * 128:(ti + 1) * 128], wd[:, fc, dt * 512:(dt + 1) * 512],
                                                                         start=(fc == 0), stop=(fc == nfc - 1)), reads=[Tact, Twd], writes=[TB[b]])
            for dt in range(2):
                b = bs_[dt]
                xs = xt[:, tile, dt * 512:(dt + 1) * 512]
                rd = [TB[b], TB[bs_[1]], Tx[tile]]
                if gate_col is None:
                    P.op("dve", lambda e, b=b, xs=xs: e.tensor_tensor(out=xs, in0=banks[b][:], in1=xs, op=ALU.add),
                         reads=rd, writes=[Tx[tile]])
                else:
                    gsc = gates[:, tile, gate_col:gate_col + 1]
                    P.op("dve", lambda e, b=b, xs=xs, gsc=gsc: e.scalar_tensor_tensor(out=xs, in0=banks[b][:], scalar=gsc, in1=xs, op0=ALU.mult, op1=ALU.add),
                         reads=rd + [Tgates], writes=[Tx[tile]])

    def ffn_flush():
        if ffn_state["pending"] is not None:
            ffn_down(ffn_state["pending"])
            ffn_state["pending"] = None

    def ffn(wg_d, wu_d, wd_d, Fdim, gate_col):
        gb = [0, 1]; ub = [2, 3]
        nst = (Fdim + 511) // 512
        for st in range(nst):
            f0 = st * 512
            nf = min(512, Fdim - f0)
            nfc = nf // 128
            wg, Twg = rload(wg_d[:, f0:f0 + nf].rearrange("(k p) c -> p k c", p=128), 8, nf)
            wu, Twu = rload(wu_d[:, f0:f0 + nf].rearrange("(k p) c -> p k c", p=128), 8, nf)
            wd, Twd = rload(wd_d[f0:f0 + nf, :].rearrange("(j p) d -> p j d", p=128), nfc, D)
            def scale_wd(j):
                P.op("dve", lambda e: e.tensor_tensor(out=wd[:, j, :], in0=wd[:, j, :], in1=gate_bc[:], op=ALU.mult),
                     reads=[Twd, Tgate], writes=[Twd])
            for tt in range(4):
                ai = ffn_state["cnt"] % 2
                ffn_state["cnt"] += 1
                actb, Tact = A4[ai], TA4[ai]
                for fc in range(nfc):
                    c2 = ffn_state.setdefault("c2", 0)
                    ffn_state["c2"] = c2 + 1
                    g = gb[c2 % 2]; u = ub[c2 % 2]
                    sg, Tsg = F5[c2 % 2], TF5[c2 % 2]
                    for k in range(8):
                        P.op("pe", lambda e, g=g, k=k, fc=fc, wg=wg, tt=tt: e.matmul(banks[g][:], wg[:, k, fc * 128:(fc + 1) * 128], hT[:, k, tt * 512:(tt + 1) * 512],
                                                                                  start=(k == 0), stop=(k == 7)), reads=[Twg, Th[tt]], writes=[TB[g]])
                    for k in range(8):
                        P.op("pe", lambda e, u=u, k=k, fc=fc, wu=wu, tt=tt: e.matmul(banks[u][:], wu[:, k, fc * 128:(fc + 1) * 128], hT[:, k, tt * 512:(tt + 1) * 512],
                                                                                  start=(k == 0), stop=(k == 7)), reads=[Twu, Th[tt]], writes=[TB[u]])
                    P.op("act", lambda e, g=g, sg=sg: e.activation(out=sg[:], in_=banks[g][:], func=AF.Silu), reads=[TB[g], TB[u]], writes=[Tsg])
                    P.op("dve", lambda e, u=u, sg=sg, actb=actb, fc=fc: e.tensor_tensor(out=actb[:, fc, :], in0=sg[:], in1=banks[u][:], op=ALU.mult),
                         reads=[Tsg, TB[u]], writes=[Tact])
                    if tt == 0 and fc == nfc - 1:
                        scale_wd(0)
                    if tt == 1 and fc + 1 < nfc:
                        scale_wd(fc + 1)
                ffn_flush()
                ffn_state["pending"] = (actb, Tact, wd, Twd, nfc, tt, gate_col)

    def router(j):
        P.op("pool", lambda e: e.dma_start(out=wrs[:], in_=wr_d[j].rearrange("(k p) c -> p k c", p=128)), writes=[Twr], dma_out=Twr)
        b = nb()
        for i in range(NT):
            for k in range(8):
                P.op("pe", lambda e, i=i, k=k, b=b: e.matmul(banks[b][:, i * 8:(i + 1) * 8], hT[:, k, i * 128:(i + 1) * 128], wrs[:, k, :], start=(k == 0), stop=(k == 7)),
                     reads=[Th[i // 4], Twr], writes=[TB[b]])
        P.op("dve", lambda e, b=b: e.tensor_copy(out=lg[:], in_=banks[b][:, 0:128].rearrange("p (i c) -> p i c", c=8)), reads=[TB[b]], writes=[Tlg])
        m1 = m12[:, 0, :]; m2 = m12[:, 1, :]; den = m12[:, 2, :]
        bc = lambda a: a.unsqueeze(2).to_broadcast([128, NT, 8])
        P.op("dve", lambda e: e.tensor_reduce(out=m1, in_=lg[:], axis=AX.X, op=ALU.max), reads=[Tlg], writes=[Tlg])
        P.op("dve", lambda e: e.tensor_tensor(out=lg2, in0=lg[:], in1=bc(m1), op=ALU.is_equal), reads=[Tlg], writes=[Tlg, TE2])
        P.op("dve", lambda e: e.scalar_tensor_tensor(out=lg2, in0=lg2, scalar=-1e30, in1=lg[:], op0=ALU.mult, op1=ALU.add), reads=[Tlg, TE2], writes=[Tlg, TE2])
        P.op("dve", lambda e: e.tensor_reduce(out=m2, in_=lg2, axis=AX.X, op=ALU.max), reads=[Tlg, TE2], writes=[Tlg])
        P.op("dve", lambda e: e.tensor_tensor(out=lg2, in0=lg[:], in1=bc(m2), op=ALU.is_ge), reads=[Tlg], writes=[Tlg, TE2])
        P.op("dve", lambda e: e.tensor_tensor(out=lg[:], in0=lg[:], in1=bc(m1), op=ALU.subtract), reads=[Tlg], writes=[Tlg])
        P.op("act", lambda e: e.activation(out=lg[:], in_=lg[:], func=AF.Exp), reads=[Tlg], writes=[Tlg])
        P.op("dve", lambda e: e.tensor_tensor(out=lg[:], in0=lg[:], in1=lg2, op=ALU.mult), reads=[Tlg, TE2], writes=[Tlg])
        P.op("dve", lambda e: e.tensor_reduce(out=den, in_=lg[:], axis=AX.X, op=ALU.add), reads=[Tlg], writes=[Tlg])
        P.op("dve", lambda e: e.reciprocal(out=den, in_=den), reads=[Tlg], writes=[Tlg])
        P.op("dve", lambda e: e.tensor_tensor(out=gates[:], in0=lg[:], in1=bc(den), op=ALU.mult), reads=[Tlg], writes=[Tgates])

    gam = [1.0 - 2.0 ** (-5.0 - h) for h in range(4)]

    def mixer(l):
        win = win_d[l]
        qin, Tqin = A4[0], TA4[0]
        kT, TkT = A4[1], TA4[1]
        rv, Trv = A4[2], TA4[2]
        rg, Trg = A4[3], TA4[3]
        gv, Tgv = A4[4], TA4[4]
        gr, Tgr = A4[5], TA4[5]
        gq, Tgq = F1[0], TF1[0]
        gk, Tgk = F1[1], TF1[1]
        gq3 = gq[:].rearrange("p (a t) -> p a t", t=512)
        gk3 = gk[:].rearrange("p (a t) -> p a t", t=512)
        t1, Tt1 = F5[0], TF5[0]
        t2, Tt2 = F5[1], TF5[1]
        glv = F1[2][:].bitcast(BF16)
        PTgv = glv[:, 0:512]
        sqg = glv[:, 512:1024]
        kitmv = glv[:, 1024:1280]
        Tgl = TF1[2]
        P.op("sp", lambda e: e.dma_start(out=w2s[:], in_=w2_d[l]), writes=[Tw2], dma_out=Tw2)
        P.op("sp", lambda e: e.dma_start(out=b2s[:], in_=b2_d[l:l + 1, :]), writes=[Tw2], dma_out=Tw2)
        P.op("dve", lambda e: e.memset(Sr[:], 0.0), writes=[TSr])
        P.op("dve", lambda e: e.memset(Srb[:], 0.0), writes=[TSrb])
        P.op("dve", lambda e: e.memset(Sg[:], 0.0), writes=[TSg])

        def panel(c0, nc_):
            return rload(win[:, c0:c0 + nc_].rearrange("(k p) c -> p k c", p=128), 8, nc_)

        def fm_proj(pan, Tp, col, tt):
            b = nb()
            for k in range(8):
                P.op("pe", lambda e, b=b, k=k: e.matmul(banks[b][:], pan[:, k, col * 128:(col + 1) * 128], hT[:, k, tt * 512:(tt + 1) * 512], start=(k == 0), stop=(k == 7)),
                     reads=[Tp, Th[tt]], writes=[TB[b]])
            return b

        def tm_proj(pan, Tp, tt, dst, Tdst):
            for ti in range(4):
                b = nb()
                t0 = tt * 512 + ti * 128
                for k in range(8):
                    P.op("pe", lambda e, b=b, k=k, t0=t0: e.matmul(banks[b][:], hT[:, k, t0:t0 + 128], pan[:, k, :], start=(k == 0), stop=(k == 7)),
                         reads=[Tp, Th[tt]], writes=[TB[b]])
                P.op("act", lambda e, b=b, ti=ti: e.copy(out=dst[:, ti, :], in_=banks[b][:]), reads=[TB[b]], writes=[Tdst])

        for tt in range(4):
            tsl = slice(tt * 512, (tt + 1) * 512)
            for (c0, dst, Tdst, isq) in ((0, qin, Tqin, True), (1024, kT, TkT, False)):
                pa, Tpa = panel(c0, 512)
                pb, Tpb = panel(c0 + 512, 512)
                for h in range(4):
                    b1 = fm_proj(pa, Tpa, h, tt)
                    b2 = fm_proj(pb, Tpb, h, tt)
                    P.op("dve", lambda e, b1=b1: e.tensor_tensor(out=t1[:], in0=banks[b1][:], in1=cosT[:, tsl], op=ALU.mult), reads=[TB[b1], Ttab], writes=[Tt1])
                    P.op("dve", lambda e, b2=b2: e.tensor_tensor(out=t2[:], in0=banks[b2][:], in1=sinT[:, tsl], op=ALU.mult), reads=[TB[b2], Ttab], writes=[Tt2])
                    if isq:
                        P.op("dve", lambda e: e.tensor_tensor(out=t1[:], in0=t1[:], in1=t2[:], op=ALU.add), reads=[Tt1, Tt2], writes=[Tt1])
                        P.op("dve", lambda e, h=h: e.tensor_tensor(out=dst[:, h, :].rearrange("p (n c) -> p n c", c=128),
                                                                   in0=t1[:].rearrange("p (n c) -> p n c", c=128),
                                                                   in1=gqtab[:, h * 128:(h + 1) * 128].unsqueeze(1).to_broadcast([128, 4, 128]), op=ALU.mult),
                             reads=[Tt1, Ttab], writes=[Tdst])
                    else:
                        P.op("dve", lambda e, h=h: e.tensor_tensor(out=dst[:, h, :], in0=t1[:], in1=t2[:], op=ALU.add), reads=[Tt1, Tt2], writes=[Tdst])
            pa, Tpa = panel(2048, 512)
            tm_proj(pa, Tpa, tt, rv, Trv)
            pa, Tpa = panel(2560, 512)
            for h in range(4):
                b = fm_proj(pa, Tpa, h, tt)
                P.op("act", lambda e, b=b, h=h: e.activation(out=rg[:, h, :], in_=banks[b][:], func=AF.Silu), reads=[TB[b]], writes=[Trg])
            pa, Tpa = panel(3072, 512)
            for a in range(2):
                b = fm_proj(pa, Tpa, a, tt)
                P.op("act", lambda e, b=b, a=a: e.copy(out=gq3[:, a, :], in_=banks[b][:]), reads=[TB[b]], writes=[Tgq])
            for a in range(2):
                b = fm_proj(pa, Tpa, 2 + a, tt)
                P.op("act", lambda e, b=b, a=a: e.copy(out=gk3[:, a, :], in_=banks[b][:]), reads=[TB[b]], writes=[Tgk])
            pa, Tpa = panel(3584, 512)
            tm_proj(pa, Tpa, tt, gv, Tgv)
            pa, Tpa = panel(4096, 512)
            for h in range(4):
                b = fm_proj(pa, Tpa, h, tt)
                P.op("act", lambda e, b=b, h=h: e.activation(out=gr[:, h, :], in_=banks[b][:], func=AF.Silu), reads=[TB[b]], writes=[Tgr])
            if mix_stage < 1:
                continue
            pa, Tpa = panel(4608, 16)
            b = nb()
            for k in range(8):
                P.op("pe", lambda e, b=b, k=k: e.matmul(banks[b][0:16, :], pa[:, k, 0:16], hT[:, k, tsl], start=(k == 0), stop=(k == 7)),
                     reads=[Tpa, Th[tt]], writes=[TB[b]])
            P.op("act", lambda e, b=b: e.copy(out=gaT[0:16, :], in_=banks[b][0:16, :]), reads=[TB[b]], writes=[TgaT])

            def ret_chunk(n):
                csl = slice(n * 128, (n + 1) * 128)
                bS = 0
                for h in range(4):
                    P.op("pe", lambda e, h=h: e.matmul(banks[bS][:, h * 128:(h + 1) * 128], kT[:, h, csl], qin[:, h, csl], start=True, stop=True),
                         reads=[TkT, Tqin], writes=[TB[bS]])
                yield
                P.op("dve", lambda e: e.tensor_tensor(out=PTr[:], in0=banks[bS][:], in1=maskR[:], op=ALU.mult), reads=[TB[bS], Ttab], writes=[TPTr])
                yield
                bO = 1
                for h in range(4):
                    hs = slice(h * 128, (h + 1) * 128)
                    P.op("pe", lambda e, hs=hs: e.matmul(banks[bO][:, hs], rv[:, n, hs], PTr[:, hs], start=True, stop=False),
                         reads=[Trv, TPTr], writes=[TB[bO]])
                    P.op("pe", lambda e, hs=hs, h=h: e.matmul(banks[bO][:, hs], Srb[:, hs], qin[:, h, csl], start=False, stop=True),
                         reads=[TSrb, Tqin], writes=[TB[bO]])
                yield
                bK = 2
                bKv = banks[bK][:].bitcast(BF16)
                for h in range(4):
                    P.op("pe", lambda e, h=h: e.transpose(out=bKv[:, h * 128:(h + 1) * 128], in_=kT[:, h, csl], identity=idb[:]),
                         reads=[TkT, Tidb], writes=[TB[bK]])
                yield
                P.op("dve", lambda e: e.tensor_tensor(out=kst[:], in0=bKv[:, 0:512], in1=kdec[:], op=ALU.mult), reads=[TB[bK], Ttab], writes=[Tkst])
                yield
                bC = 3
                for h in range(4):
                    hs = slice(h * 128, (h + 1) * 128)
                    P.op("pe", lambda e, hs=hs: e.matmul(banks[bC][:, hs], kst[:, hs], rv[:, n, hs], start=True, stop=True),
                         reads=[Tkst, Trv], writes=[TB[bC]])
                yield
                for h in range(4):
                    hs = slice(h * 128, (h + 1) * 128)
                    P.op("dve", lambda e, hs=hs, h=h: e.scalar_tensor_tensor(out=Sr[:, hs], in0=Sr[:, hs], scalar=float(gam[h] ** 128), in1=banks[bC][:, hs], op0=ALU.mult, op1=ALU.add),
                         reads=[TSr, TB[bC]], writes=[TSr])
                yield
                P.op("act", lambda e: e.copy(out=Srb[:], in_=Sr[:]), reads=[TSr], writes=[TSrb])
                yield
                yield from head_norm(bO, 0, rg, Trg, 0, tt, n, F5[0], TF5[0], sq[:], Tsq)

            def gla_chunk(n):
                csl = slice(n * 128, (n + 1) * 128)
                bL = 4
                P.op("pe", lambda e: e.matmul(banks[bL][:, 0:256], gaT[0:16, csl], w2s[:], start=True, stop=False), reads=[TgaT, Tw2], writes=[TB[bL]])
                P.op("pe", lambda e: e.matmul(banks[bL][:, 0:256], ones_row[0:1, :], b2s[0:1, :], start=False, stop=True), reads=[Tones, Tw2], writes=[TB[bL]])
                yield
                P.op("act", lambda e: e.activation(out=esp[:], in_=banks[bL][:, 0:256], func=AF.Exp, scale=-1.0), reads=[TB[bL]], writes=[Tesp])
                yield
                P.op("act", lambda e: e.activation(out=esp[:], in_=esp[:], func=AF.Ln, bias=1.0), reads=[Tesp], writes=[Tesp])
                yield
                bB = 5
                for a in range(2):
                    P.op("pe", lambda e, a=a: e.matmul(banks[bB][:, a * 128:(a + 1) * 128], esp[:, a * 128:(a + 1) * 128], tri[:], start=True, stop=True),
                         reads=[Tesp, Tconst], writes=[TB[bB]])
                yield
                bT3 = banks[bB][:, 0:256].rearrange("p (a t) -> p a t", t=128)
                P.op("dve", lambda e: e.tensor_copy(out=sm[:, 0:2], in_=bT3[:, :, 63]), reads=[TB[bB]], writes=[Tsm])
                P.op("dve", lambda e: e.tensor_scalar(out=sm[:, 2:4], in0=bT3[:, :, 63], scalar1=-1.0, scalar2=None, op0=ALU.mult), reads=[TB[bB]], writes=[Tsm])
                P.op("dve", lambda e: e.tensor_copy(out=sm[:, 4:6], in_=bT3[:, :, 127]), reads=[TB[bB]], writes=[Tsm])
                yield
                P.op("dve", lambda e: e.tensor_tensor(out=sm[:, 12:14], in0=sm[:, 4:6], in1=sm[:, 0:2], op=ALU.subtract), reads=[Tsm], writes=[Tsm])
                yield
                for a in range(2):
                    asl = slice(a * 128, (a + 1) * 128)
                    P.op("act", lambda e, a=a, asl=asl: e.activation(out=E1[:, asl], in_=banks[bB][:, asl], func=AF.Exp, bias=sm[:, 2 + a:3 + a], scale=1.0),
                         reads=[TB[bB], Tsm], writes=[TE1])
                    P.op("act", lambda e, a=a, asl=asl: e.activation(out=E2[:, asl], in_=banks[bB][:, asl], func=AF.Exp, bias=sm[:, a:a + 1], scale=-1.0),
                         reads=[TB[bB], Tsm], writes=[TE2])
                yield
                P.op("act", lambda e: e.activation(out=sm[:, 6:8], in_=sm[:, 4:6], func=AF.Exp), reads=[Tsm], writes=[Tsm])
                P.op("act", lambda e: e.activation(out=sm[:, 8:10], in_=sm[:, 12:14], func=AF.Exp), reads=[Tsm], writes=[Tsm])
                P.op("act", lambda e: e.activation(out=sm[:, 10:12], in_=sm[:, 0:2], func=AF.Exp), reads=[Tsm], writes=[Tsm])
                yield
                for X in range(2):
                    P.op("dve", lambda e, X=X: e.scalar_tensor_tensor(out=qi2[:, X, :, :], in0=gq3[:, :, csl], scalar=rmask[:, X:X + 1],
                                                                      in1=E1[:].rearrange("p (a t) -> p a t", t=128), op0=ALU.mult, op1=ALU.mult),
                         reads=[Tgq, TE1, Tconst], writes=[Tqi])
                P.op("dve", lambda e: e.tensor_tensor(out=ki[:], in0=gk3[:, :, csl], in1=E2[:].rearrange("p (a t) -> p a t", t=128), op=ALU.mult),
                     reads=[Tgk, TE2], writes=[Tki])
                yield
                bS = 4
                for h in range(4):
                    a = h // 2
                    P.op("pe", lambda e, h=h, a=a: e.matmul(banks[bS][:, h * 128:(h + 1) * 128], ki[:, a, :], qi2[:, h % 2, a, :], start=True, stop=True),
                         reads=[Tki, Tqi], writes=[TB[bS]])
                yield
                P.op("dve", lambda e: e.tensor_tensor(out=PTgv, in0=banks[bS][:], in1=maskG[:], op=ALU.mult), reads=[TB[bS], Ttab], writes=[Tgl])
                for a in range(2):
                    P.op("dve", lambda e, a=a: e.tensor_scalar(out=Sgb[:, a, :], in0=Sg[:, a, :], scalar1=sm[:, 10 + a:11 + a], scalar2=0.125, op0=ALU.mult, op1=ALU.mult),
                         reads=[TSg, Tsm], writes=[TSgb])
                yield
                bO = 6
                for h in range(4):
                    a = h // 2
                    hs = slice(h * 128, (h + 1) * 128)
                    P.op("pe", lambda e, hs=hs: e.matmul(banks[bO][:, hs], gv[:, n, hs], PTgv[:, hs], start=True, stop=False),
                         reads=[Tgv, Tgl], writes=[TB[bO]])
                    P.op("pe", lambda e, hs=hs, a=a, h=h: e.matmul(banks[bO][:, hs], Sgb[:, a, :], qi2[:, h % 2, a, :], start=False, stop=True),
                         reads=[TSgb, Tqi], writes=[TB[bO]])
                yield
                bK = 7
                bKv = banks[bK][:].bitcast(BF16)
                for a in range(2):
                    P.op("pe", lambda e, a=a: e.transpose(out=bKv[:, a * 128:(a + 1) * 128], in_=ki[:, a, :], identity=idb[:]),
                         reads=[Tki, Tidb], writes=[TB[bK]])
                yield
                P.op("act", lambda e: e.copy(out=kitmv, in_=bKv[:, 0:256]), reads=[TB[bK]], writes=[Tgl])
                yield
                bC = 5
                for a in range(2):
                    P.op("pe", lambda e, a=a: e.matmul(banks[bC][:, a * 256:(a + 1) * 256], kitmv[:, a * 128:(a + 1) * 128], gv[:, n, a * 256:(a + 1) * 256], start=True, stop=True),
                         reads=[Tgl, Tgv], writes=[TB[bC]])
                yield
                for a in range(2):
                    for hh in range(2):
                        ps_ = slice(64 * hh, 64 * hh + 64)
                        cs0 = a * 256 + hh * 128
                        P.op("dve", lambda e, a=a, ps_=ps_, cs0=cs0: e.tensor_scalar(out=tmpc[ps_, a, :], in0=banks[bC][ps_, cs0:cs0 + 128], scalar1=sm[ps_, 8 + a:9 + a], scalar2=None, op0=ALU.mult),
                             reads=[TB[bC], Tsm], writes=[Ttmpc])
                yield
                for a in range(2):
                    P.op("dve", lambda e, a=a: e.scalar_tensor_tensor(out=Sg[:, a, :], in0=Sg[:, a, :], scalar=sm[:, 6 + a:7 + a], in1=tmpc[:, a, :], op0=ALU.mult, op1=ALU.add),
                         reads=[TSg, Tsm, Ttmpc], writes=[TSg])
                yield
                yield from head_norm(bO, 4, gr, Tgr, 4, tt, n, F5[2], TF5[2], sqg, Tgl)

            if mix_stage >= 2:
                for n in range(4):
                    active = [ret_chunk(n), gla_chunk(n)]
                    while active:
                        for g in list(active):
                            try:
                                next(g)
                            except StopIteration:
                                active.remove(g)

            if mix_stage < 8:
                continue
            for dt in range(2):
                wo, Two = rload(wout_d[l][:, dt * 512:(dt + 1) * 512].rearrange("(k p) c -> p k c", p=128), 8, 512)
                for k in range(8):
                    P.op("dve", lambda e, wo=wo, k=k, dt=dt: e.tensor_tensor(out=wo[:, k, :], in0=wo[:, k, :], in1=gate_bc[:, dt * 512:(dt + 1) * 512], op=ALU.mult),
                         reads=[Two, Tgate], writes=[Two])
                for ti in range(4):
                    tile = tt * 4 + ti
                    b = nb()
                    t0 = tt * 512 + ti * 128
                    for k in range(8):
                        P.op("pe", lambda e, b=b, k=k, t0=t0, wo=wo: e.matmul(banks[b][:], hT[:, k, t0:t0 + 128], wo[:, k, :], start=(k == 0), stop=(k == 7)),
                             reads=[Th[tt], Two], writes=[TB[b]])
                    xs = xt[:, tile, dt * 512:(dt + 1) * 512]
                    P.op("dve", lambda e, b=b, xs=xs: e.tensor_tensor(out=xs, in0=banks[b][:], in1=xs, op=ALU.add), reads=[TB[b], Tx[tile]], writes=[Tx[tile]])

    def head_norm(bO, bs, gT, TgT, k0, tt, n, Abuf, TA, sqb, Tsqb):
        P.op("act", lambda e: e.activation(out=sqb, in_=banks[bO][:], func=AF.Square), reads=[TB[bO]], writes=[Tsqb])
        yield
        P.op("pe", lambda e: e.matmul(banks[bs][:], onesb[:], sqb, start=True, stop=True), reads=[Tones, Tsqb], writes=[TB[bs]])
        yield
        P.op("act", lambda e: e.activation(out=Abuf[:], in_=banks[bs][:], func=AF.Ln, bias=EPS, scale=1.0), reads=[TB[bs]], writes=[TA])
        yield
        P.op("act", lambda e: e.activation(out=Abuf[:], in_=Abuf[:], func=AF.Exp, scale=-0.5), reads=[TA], writes=[TA])
        yield
        P.op("dve", lambda e: e.tensor_tensor(out=Abuf[:], in0=banks[bO][:], in1=Abuf[:], op=ALU.mult), reads=[TB[bO], TA], writes=[TA])
        yield
        t0 = tt * 512 + n * 128
        P.op("dve", lambda e: e.tensor_tensor(out=hT[:, k0:k0 + 4, t0:t0 + 128], in0=Abuf[:].rearrange("p (h c) -> p h c", c=128),
                                              in1=gT[:, :, n * 128:(n + 1) * 128], op=ALU.mult), reads=[TA, TgT], writes=[Th[tt]])
        yield

    for l in (layers if layers is not None else range(depth)):
        if do_mixer:
            norm_to_hT(l, 0)
            mixer(l)
        if do_ffn:
            norm_to_hT(l, 1)
            j = l // 2
            if l % 2 == 0:
                ffn(dwg_d[j], dwu_d[j], dwd_d[j], 2816, None)
                ffn_flush()
            else:
                router(j)
                for ex in range(8):
                    ffn(mwg_d[j, ex], mwu_d[j, ex], mwd_d[j, ex], 3584, ex)
                ffn_flush()

    gfb, Tgfb = F1[0], TF1[0]
    P.op("sp", lambda e: e.dma_start(out=gfb[:], in_=gfin_d.to_broadcast([128, D])), writes=[Tgfb], dma_out=Tgfb)
    rms_stats()
    for i in range(NT):
        ob, Tob = F1[1 + i % 2], TF1[1 + i % 2]
        P.op("dve", lambda e, i=i, ob=ob: e.scalar_tensor_tensor(out=ob[:], in0=xt[:, i, :], scalar=rstd[:, i:i + 1], in1=gfb[:], op0=ALU.mult, op1=ALU.mult),
             reads=[Tx[i], Trstd, Tgfb], writes=[Tob])
        P.op("sp", lambda e, i=i, ob=ob: e.dma_start(out=out_d[i * 128:(i + 1) * 128, :], in_=ob[:]), reads=[Tob], writes=[Tout], dma_out=Tout)
    P.op("sp", lambda e: e.nop(), reads=[Tout] + TF1)
    P.run()
    return nc


def _consts():
    f = np.float32
    idf = np.eye(128, dtype=f)
    s = np.arange(128)
    tri = np.where(s[:, None] <= s[None, :], -1.0 / 16.0, 0.0).astype(f)
    d = np.arange(128)
    j = d % 64
    inv = (10000.0 ** (-(j.astype(np.float64)) / 64.0))
    t = np.arange(S, dtype=np.float64)
    ang = inv[:, None] * t[None, :]
    cos = np.cos(ang).astype(f)
    sin = np.sin(ang)
    sin = np.where(d[:, None] < 64, -sin, sin).astype(f)
    gam = np.array([1.0 - 2.0 ** (-5.0 - h) for h in range(4)], dtype=np.float64)
    maskR = np.zeros((128, 512), f)
    kdec = np.zeros((128, 512), f)
    gqtab = np.zeros((128, 512), f)
    maskG = np.zeros((128, 512), f)
    causal = (s[:, None] <= s[None, :])
    for h in range(4):
        maskR[:, h * 128:(h + 1) * 128] = np.where(causal, (gam[h] ** (-(s[:, None] + 1.0))) * (128.0 ** -0.5), 0.0)
        kdec[:, h * 128:(h + 1) * 128] = ((gam[h] ** (127.0 - s)) * (128.0 ** -0.5))[:, None]
        gqtab[:, h * 128:(h + 1) * 128] = (gam[h] ** (s + 1.0))[None, :]
        maskG[:, h * 128:(h + 1) * 128] = np.where(causal, 64.0 ** -0.5, 0.0)
    rmask = np.zeros((128, 2), f)
    rmask[:64, 0] = 1.0
    rmask[64:, 1] = 1.0
    return dict(idf=idf, tri=tri, cos=cos, sin=sin, maskR=maskR, kdec=kdec, gqtab=gqtab, maskG=maskG, rmask=rmask)


def _win_cols():
    sw = np.concatenate([h * 128 + (np.arange(128) + 64) % 128 for h in range(4)])
    rq = np.arange(0, 512); rk = np.arange(512, 1024); rv = np.arange(1024, 1536); rg = np.arange(1536, 2048)
    gq = np.arange(2048, 2304); gk = np.arange(2304, 2560); gv = np.arange(2560, 3072); gr = np.arange(3072, 3584)
    ga = np.arange(3584, 3600)
    return np.concatenate([rq, rq[sw], rk, rk[sw], rv, rg, gq, gk, gv, gr, ga])


def make_in_maps(inputs, n_cores=8):
    f = np.float32
    g = lambda k: np.ascontiguousarray(np.asarray(inputs[k], dtype=f))
    shared = dict(
        ada_w=g("ada_w"), ada_b=g("ada_b"), gmix=g("norm_mix_g"), gffn=g("norm_ffn_g"),
        gfin=g("final_g").reshape(1, D),
        w_in=np.ascontiguousarray(g("w_in")[:, :, _win_cols()]),
        w2=g("w_gla_gate2"), b2=g("b_gla_gate"), w_out=g("w_out"),
        dwg=g("dense_w_gate"), dwu=g("dense_w_up"), dwd=g("dense_w_down"),
        wr=g("w_router"), mwg=g("moe_w_gate"), mwu=g("moe_w_up"), mwd=g("moe_w_down"),
    )
    shared.update(_consts())
    x = g("x")
    c = g("c")
    maps = []
    for b in range(n_cores):
        m = dict(shared)
        m["x"] = np.ascontiguousarray(x[b])
        m["c"] = np.ascontiguousarray(c[b].reshape(8, 128).T)
        maps.append(m)
    return maps


_NC = None


def kernel(**inputs):
    global _NC
    if _NC is None:
        _NC = build()
    maps = make_in_maps(inputs, 8)
    res = run_bass_kernel_spmd(_NC, maps, core_ids=list(range(8)))
    return np.stack([np.asarray(r["out"], dtype=np.float32) for r in res.results], axis=0)
```

```python
import math
import types
import numpy as np
import concourse.bass as bass
import concourse.mybir as mybir
from concourse.bass_utils import run_bass_kernel_spmd

F32 = mybir.dt.float32
BF16 = mybir.dt.bfloat16
AF = mybir.ActivationFunctionType
ALU = mybir.AluOpType
AX = mybir.AxisListType

ENGS = ("pe", "act", "dve", "pool", "sp")
EPOCH = 8000

D = 1024
S = 2048
NT = 16
EPS = 1e-6
WIN_COLS = 4624
PIPE_W = 3


class T:
    __slots__ = ("name", "writer", "readers", "dsem", "dcount", "excl")

    def __init__(self, name, excl=False):
        self.name = name
        self.excl = excl
        self.writer = None
        self.readers = []
        self.dsem = None
        self.dcount = 0


class Op:
    __slots__ = ("eng", "fn", "deps", "is_dma", "dma_tile", "dma_val", "sig", "sigidx", "waits", "id", "pos")


def _freeze(fn):
    if fn.__closure__ is None:
        return fn
    cells = []
    for c in fn.__closure__:
        try:
            cells.append(types.CellType(c.cell_contents))
        except ValueError:
            cells.append(c)
    return types.FunctionType(fn.__code__, fn.__globals__, fn.__name__, fn.__defaults__, tuple(cells))


class Prog:
    def __init__(self, nc):
        self.nc = nc
        self.ops = []
        self.eng_ops = {e: [] for e in ENGS}

    def op(self, eng, fn, reads=(), writes=(), dma_out=None):
        o = Op()
        o.id = len(self.ops)
        o.eng = eng
        o.fn = _freeze(fn)
        o.is_dma = False
        o.dma_tile = None
        o.dma_val = 0
        o.sig = False
        o.sigidx = -1
        o.waits = []
        deps = set()
        for t in reads:
            if t.writer is not None:
                deps.add(t.writer)
            if t.excl:
                for r in t.readers:
                    if self.ops[r].eng != eng:
                        deps.add(r)
        for t in writes:
            if t.writer is not None:
                deps.add(t.writer)
            for r in t.readers:
                deps.add(r)
        o.deps = sorted(deps)
        if dma_out is not None:
            o.is_dma = True
            o.dma_tile = dma_out
            if dma_out.dsem is None:
                dma_out.dsem = self.nc.alloc_semaphore("d_" + dma_out.name)
            dma_out.dcount += 16
            o.dma_val = dma_out.dcount
        for t in reads:
            t.readers.append(o.id)
        for t in writes:
            t.writer = o.id
            t.readers = []
        self.ops.append(o)
        self.eng_ops[eng].append(o)
        return o

    def finalize(self):
        nc = self.nc
        ops = self.ops
        for e in ENGS:
            for i, o in enumerate(self.eng_ops[e]):
                o.pos = i
        eidx = {e: i for i, e in enumerate(ENGS)}
        NE = len(ENGS)
        clock = {e: [-1] * NE for e in ENGS}
        opclock = [None] * len(ops)
        dma_known = {e: {} for e in ENGS}
        for o in ops:
            ck = clock[o.eng]
            dk = dma_known[o.eng]
            best = {}
            dmas = {}
            for d in o.deps:
                p = ops[d]
                if p.is_dma:
                    key = id(p.dma_tile)
                    if key not in dmas or dmas[key].dma_val < p.dma_val:
                        dmas[key] = p
                else:
                    if p.eng == "pe" and o.eng == "pe" and not o.is_dma:
                        continue
                    if p.eng not in best or best[p.eng].pos < p.pos:
                        best[p.eng] = p
            for key, p in dmas.items():
                if dk.get(key, 0) >= p.dma_val:
                    continue
                dk[key] = p.dma_val
                o.waits.append(p)
                pc = opclock[p.id]
                for i in range(NE):
                    if pc[i] > ck[i]:
                        ck[i] = pc[i]
            for e2, p in best.items():
                pi = eidx[e2]
                if ck[pi] >= p.pos:
                    continue
                o.waits.append(p)
                p.sig = True
                pc = opclock[p.id]
                for i in range(NE):
                    if pc[i] > ck[i]:
                        ck[i] = pc[i]
                if ck[pi] < p.pos:
                    ck[pi] = p.pos
            snap = list(ck)
            if not o.is_dma:
                snap[eidx[o.eng]] = max(snap[eidx[o.eng]], o.pos)
            opclock[o.id] = snap
        nsig = {e: 0 for e in ENGS}
        for e in ENGS:
            for o in self.eng_ops[e]:
                if o.sig and not o.is_dma:
                    o.sigidx = nsig[e]
                    nsig[e] += 1
        self.esems = {}
        for e in ENGS:
            n = (nsig[e] + EPOCH - 1) // EPOCH
            self.esems[e] = [nc.alloc_semaphore(f"s_{e}{i}") for i in range(n)]
        for o in ops:
            w = []
            for p in o.waits:
                if p.is_dma:
                    w.append((p.dma_tile.dsem, p.dma_val))
                else:
                    w.append((self.esems[p.eng][p.sigidx // EPOCH], p.sigidx % EPOCH + 1))
            o.waits = w

    def emit_engine(self, e, eng):
        for o in self.eng_ops[e]:
            for (sem, val) in o.waits:
                eng.wait_ge(sem, val)
            ins = o.fn(eng)
            if o.is_dma:
                ins.then_inc(o.dma_tile.dsem, 16)
            elif o.sigidx >= 0:
                ins.then_inc(self.esems[e][o.sigidx // EPOCH], 1)

    def run(self):
        self.finalize()
        with self.nc.Block() as block:
            @block.tensor
            def _(eng):
                self.emit_engine("pe", eng)

            @block.scalar
            def _(eng):
                self.emit_engine("act", eng)

            @block.vector
            def _(eng):
                self.emit_engine("dve", eng)

            @block.gpsimd
            def _(eng):
                self.emit_engine("pool", eng)

            @block.sync
            def _(eng):
                self.emit_engine("sp", eng)


def build(depth=4, do_mixer=True, do_ffn=True, layers=None, mix_stage=99):
    nc = bass.Bass("TRN2", target_bir_lowering=False)
    P = Prog(nc)

    def din(name, shape):
        return nc.dram_tensor(name, list(shape), F32, kind="ExternalInput").ap()

    x_d = din("x", [S, D])
    c_d = din("c", [128, 8])
    adaw_d = din("ada_w", [4, D, 6 * D])
    adab_d = din("ada_b", [4, 6 * D])
    gmix_d = din("gmix", [4, D])
    gffn_d = din("gffn", [4, D])
    gfin_d = din("gfin", [1, D])
    win_d = din("w_in", [4, D, WIN_COLS])
    w2_d = din("w2", [4, 16, 256])
    b2_d = din("b2", [4, 256])
    wout_d = din("w_out", [4, D, D])
    dwg_d = din("dwg", [2, D, 2816])
    dwu_d = din("dwu", [2, D, 2816])
    dwd_d = din("dwd", [2, 2816, D])
    wr_d = din("wr", [2, D, 8])
    mwg_d = din("mwg", [2, 8, D, 3584])
    mwu_d = din("mwu", [2, 8, D, 3584])
    mwd_d = din("mwd", [2, 8, 3584, D])
    idf_d = din("idf", [128, 128])
    tri_d = din("tri", [128, 128])
    cos_d = din("cos", [128, S])
    sin_d = din("sin", [128, S])
    maskR_d = din("maskR", [128, 512])
    kdec_d = din("kdec", [128, 512])
    gqtab_d = din("gqtab", [128, 512])
    maskG_d = din("maskG", [128, 512])
    rmask_d = din("rmask", [128, 2])
    out_d = nc.dram_tensor("out", [S, D], F32, kind="ExternalOutput").ap()

    def sb(name, shape, dt):
        return nc.alloc_sbuf_tensor("s_" + name, list(shape), dt)

    xt = sb("xt", [128, NT, D], F32)
    Tx = [T(f"x{i}") for i in range(NT)]
    hT = sb("hT", [128, 8, S], BF16)
    Th = [T(f"h{i}") for i in range(4)]
    NR = 4
    ring = [sb(f"ring{i}", [128, 4096], BF16) for i in range(NR)]
    Tring = [T(f"ring{i}") for i in range(NR)]
    ring_i = [0]

    def rload(src, a, b):
        i = ring_i[0] % NR
        ring_i[0] += 1
        view = ring[i][:, 0:a * b].rearrange("p (a b) -> p a b", b=b)
        P.op("pool", lambda e: e.dma_start(out=view, in_=src), writes=[Tring[i]], dma_out=Tring[i])
        return view, Tring[i]

    banks = [nc.alloc_psum_tensor(f"ps{i}", [128, 512], F32) for i in range(8)]
    TB = [T(f"ps{i}", excl=True) for i in range(8)]
    bank_i = [0]

    def nb():
        i = bank_i[0] % 8
        bank_i[0] += 1
        return i

    A4 = [sb(f"A4_{i}", [128, 4, 512], BF16) for i in range(6)]
    TA4 = [T(f"A4_{i}") for i in range(6)]
    F1 = [sb(f"F1_{i}", [128, 1024], F32) for i in range(3)]
    TF1 = [T(f"F1_{i}") for i in range(3)]
    F5 = [sb(f"F5_{i}", [128, 512], F32) for i in range(4)]
    TF5 = [T(f"F5_{i}") for i in range(4)]
    gate_bc = sb("gate_bc", [128, D], F32)
    Tgate = T("gate_bc")
    gaT = F5[3]
    TgaT = TF5[3]
    junk3 = A4[5][:, 0:2, :]
    Tjunk = TA4[5]
    adab_bc = [F5[2], F5[3]]
    Tadab = [TF5[2], TF5[3]]
    ss = sb("ss", [128, NT], F32)
    Tss = T("ss")
    rstd = sb("rstd", [128, NT], F32)
    Trstd = T("rstd")
    cosT = sb("cosT", [128, S], BF16)
    sinT = sb("sinT", [128, S], BF16)
    Ttab = T("tab")
    maskR = sb("maskR", [128, 512], BF16)
    kdec = sb("kdec", [128, 512], BF16)
    gqtab = sb("gqtab", [128, 512], BF16)
    maskG = sb("maskG", [128, 512], BF16)
    idf = sb("idf", [128, 128], F32)
    idb = sb("idb", [128, 128], BF16)
    tri = sb("tri", [128, 128], F32)
    rmask = sb("rmask", [128, 2], F32)
    onesb = sb("onesb", [128, 128], BF16)
    ones_row = sb("ones_row", [1, 128], F32)
    Tconst = T("const")
    Tidb = T("idb")
    ccol = sb("ccol", [128, 8], F32)
    Cbc = sb("Cbc", [128, 8, 128], BF16)
    TC = T("C")
    w2s = sb("w2s", [16, 256], F32)
    b2s = sb("b2s", [1, 256], F32)
    Tw2 = T("w2")
    wrs = sb("wrs", [128, 8, 8], BF16)
    Twr = T("wr")
    lg = sb("lg", [128, NT, 8], F32)
    gates = sb("gates", [128, NT, 8], F32)
    m12 = sb("m12", [128, 4, NT], F32)
    Tlg = T("lg")
    Tgates = T("gates")
    PTr = sb("PTr", [128, 512], BF16); TPTr = T("PTr")
    PTg = PTr; TPTg = TPTr
    kst = sb("kst", [128, 512], BF16); Tkst = T("kst")
    sq = sb("sq", [128, 512], BF16); Tsq = T("sq")
    Sr = sb("Sr", [128, 512], F32); TSr = T("Sr")
    Srb = sb("Srb", [128, 512], BF16); TSrb = T("Srb")
    Sg = sb("Sg", [128, 2, 128], F32); TSg = T("Sg")
    Sgb = sb("Sgb", [128, 2, 128], BF16); TSgb = T("Sgb")
    esp = sb("esp", [128, 256], F32); Tesp = T("esp")
    E1 = sb("E1", [128, 256], F32); TE1 = T("E1")
    E2 = sb("E2", [128, 256], F32); TE2 = T("E2")
    lg2 = E2[:, 0:128].rearrange("p (i c) -> p i c", c=8)
    qi2 = sb("qi2", [128, 2, 2, 128], BF16); Tqi = T("qi")
    ki = sb("ki", [128, 2, 128], BF16); Tki = T("ki")
    kitm = kst[:, 0:256]; Tkitm = Tkst
    tmpc = E1[:].rearrange("p (a t) -> p a t", t=128); Ttmpc = TE1
    sm = sb("sm", [128, 16], F32); Tsm = T("sm")
    Tout = T("out_store")

    P.op("sp", lambda e: e.dma_start(out=idf[:], in_=idf_d), writes=[Tconst], dma_out=Tconst)
    P.op("sp", lambda e: e.dma_start(out=tri[:], in_=tri_d), writes=[Tconst], dma_out=Tconst)
    P.op("sp", lambda e: e.dma_start(out=rmask[:], in_=rmask_d), writes=[Tconst], dma_out=Tconst)
    P.op("pool", lambda e: e.dma_start(out=idb[:], in_=idf_d), writes=[Tidb], dma_out=Tidb)
    P.op("pool", lambda e: e.dma_start(out=cosT[:], in_=cos_d), writes=[Ttab], dma_out=Ttab)
    P.op("pool", lambda e: e.dma_start(out=sinT[:], in_=sin_d), writes=[Ttab], dma_out=Ttab)
    P.op("pool", lambda e: e.dma_start(out=maskR[:], in_=maskR_d), writes=[Ttab], dma_out=Ttab)
    P.op("pool", lambda e: e.dma_start(out=kdec[:], in_=kdec_d), writes=[Ttab], dma_out=Ttab)
    P.op("pool", lambda e: e.dma_start(out=gqtab[:], in_=gqtab_d), writes=[Ttab], dma_out=Ttab)
    P.op("pool", lambda e: e.dma_start(out=maskG[:], in_=maskG_d), writes=[Ttab], dma_out=Ttab)
    Tones = T("ones")
    P.op("dve", lambda e: e.memset(onesb[:], 1.0 / 128.0), writes=[Tones])
    P.op("dve", lambda e: e.memset(ones_row[:], 1.0), writes=[Tones])
    P.op("sp", lambda e: e.dma_start(out=ccol[:], in_=c_d), writes=[TC], dma_out=TC)
    for i in range(NT):
        P.op("sp", lambda e, i=i: e.dma_start(out=xt[:, i, :], in_=x_d[i * 128:(i + 1) * 128, :]), writes=[Tx[i]], dma_out=Tx[i])
    P.op("act", lambda e: e.activation(out=ccol[:], in_=ccol[:], func=AF.Silu), reads=[TC], writes=[TC])
    P.op("dve", lambda e: e.tensor_copy(out=Cbc[:], in_=ccol[:].unsqueeze(2).to_broadcast([128, 8, 128])), reads=[TC], writes=[TC])

    def mod_tile(l, vec, dst, Tdst, kind, gsrc=None):
        if kind == "scale":
            gb, Tgb = F1[2], TF1[2]
            P.op("sp", lambda e: e.dma_start(out=gb[:], in_=gsrc.to_broadcast([128, D])), writes=[Tgb], dma_out=Tgb)
        for half in range(2):
            c0 = vec * D + half * 512
            pan, Tp = rload(adaw_d[l][:, c0:c0 + 512].rearrange("(k p) c -> p k c", p=128), 8, 512)
            ab, Tab = adab_bc[half], Tadab[half]
            P.op("sp", lambda e, ab=ab, c0=c0: e.dma_start(out=ab[:], in_=adab_d[l:l + 1, c0:c0 + 512].to_broadcast([128, 512])),
                 writes=[Tab], dma_out=Tab)
            b = nb()
            for k in range(8):
                P.op("pe", lambda e, b=b, k=k, pan=pan: e.matmul(banks[b][:], Cbc[:, k, :], pan[:, k, :], start=(k == 0), stop=(k == 7)),
                     reads=[TC, Tp], writes=[TB[b]])
            dsl = dst[:, half * 512:(half + 1) * 512]
            P.op("dve", lambda e, b=b, dsl=dsl, ab=ab: e.tensor_tensor(out=dsl, in0=banks[b][:], in1=ab[:], op=ALU.add),
                 reads=[TB[b], Tab], writes=[Tdst])
            if kind == "scale":
                gsl = gb[:, half * 512:(half + 1) * 512]
                P.op("dve", lambda e, dsl=dsl, gsl=gsl: e.scalar_tensor_tensor(out=dsl, in0=dsl, scalar=1.0, in1=gsl, op0=ALU.add, op1=ALU.mult),
                     reads=[Tdst, Tgb], writes=[Tdst])

    def rms_stats():
        P.op("dve", lambda e: e.memset(ss[:], 0.0), writes=[Tss])
        for i in range(NT):
            P.op("act", lambda e, i=i: e.activation(out=junk3, in_=xt[:, i, :].rearrange("p (a b) -> p a b", b=512), func=AF.Square, accum_out=ss[:, i:i + 1]),
                 reads=[Tx[i]], writes=[Tjunk, Tss])
        P.op("act", lambda e: e.activation(out=rstd[:], in_=ss[:], func=AF.Ln, bias=EPS, scale=1.0 / D), reads=[Tss], writes=[Trstd])
        P.op("act", lambda e: e.activation(out=rstd[:], in_=rstd[:], func=AF.Exp, scale=-0.5), reads=[Trstd], writes=[Trstd])

    def norm_to_hT(l, which):
        gs, Tgs = F1[0], TF1[0]
        sh, Tsh = F1[1], TF1[1]
        gsrc = (gmix_d if which == 0 else gffn_d)[l:l + 1, :]
        rms_stats()
        mod_tile(l, 3 * which + 0, sh, Tsh, "plain")
        mod_tile(l, 3 * which + 1, gs, Tgs, "scale", gsrc)
        xnb = [(F1[2][:], TF1[2]), (A4[2][:].bitcast(F32).rearrange("p a b -> p (a b)"), TA4[2])]
        for i in range(NT):
            xn, Txn = xnb[i % 2]
            P.op("dve", lambda e, i=i: e.scalar_tensor_tensor(out=xn, in0=xt[:, i, :], scalar=rstd[:, i:i + 1], in1=gs[:],
                                                              op0=ALU.mult, op1=ALU.mult), reads=[Tx[i], Trstd, Tgs], writes=[Txn])
            P.op("dve", lambda e: e.tensor_tensor(out=xn, in0=xn, in1=sh[:], op=ALU.add), reads=[Txn, Tsh], writes=[Txn])
            for half in range(2):
                b = nb()
                for j in range(4):
                    k = half * 4 + j
                    P.op("pe", lambda e, b=b, j=j, k=k: e.transpose(out=banks[b][:, j * 128:(j + 1) * 128], in_=xn[:, k * 128:(k + 1) * 128], identity=idf[:]),
                         reads=[Txn, Tconst], writes=[TB[b]])
                P.op("act", lambda e, b=b, half=half, i=i: e.copy(out=hT[:, half * 4:half * 4 + 4, i * 128:(i + 1) * 128],
                                                                   in_=banks[b][:].rearrange("p (j t) -> p j t", t=128)),
                     reads=[TB[b]], writes=[Th[i // 4]])
        mod_tile(l, 3 * which + 2, gate_bc, Tgate, "plain")

    ffn_state = {"cnt": 0, "dcnt": 0, "pending": None}

    def ffn_down(args):
        actb, Tact, wd, Twd, nfc, tt, gate_col = args
        db = [4, 5, 6, 7]
        for ti in range(4):
            tile = tt * 4 + ti
            bs_ = []
            for dt in range(2):
                b = db[ffn_state["dcnt"] % 4]
                ffn_state["dcnt"] += 1
                bs_.append(b)
                for fc in range(nfc):
                    P.op("pe", lambda e, b=b, fc=fc, ti=ti, dt=dt: e.matmul(banks[b][:], actb[:, fc, ti * 128:(ti + 1) * 128], wd[:, fc, dt * 512:(dt + 1) * 512],
                                                                         start=(fc == 0), stop=(fc == nfc - 1)), reads=[Tact, Twd], writes=[TB[b]])
            for dt in range(2):
                b = bs_[dt]
                xs = xt[:, tile, dt * 512:(dt + 1) * 512]
                rd = [TB[b], TB[bs_[1]], Tx[tile]]
                if gate_col is None:
                    P.op("dve", lambda e, b=b, xs=xs: e.tensor_tensor(out=xs, in0=banks[b][:], in1=xs, op=ALU.add),
                         reads=rd, writes=[Tx[tile]])
                else:
                    gsc = gates[:, tile, gate_col:gate_col + 1]
                    P.op("dve", lambda e, b=b, xs=xs, gsc=gsc: e.scalar_tensor_tensor(out=xs, in0=banks[b][:], scalar=gsc, in1=xs, op0=ALU.mult, op1=ALU.add),
                         reads=rd + [Tgates], writes=[Tx[tile]])

    def ffn_flush():
        if ffn_state["pending"] is not None:
            ffn_down(ffn_state["pending"])
            ffn_state["pending"] = None

    def ffn(wg_d, wu_d, wd_d, Fdim, gate_col):
        gb = [0, 1]; ub = [2, 3]
        nst = (Fdim + 511) // 512
        for st in range(nst):
            f0 = st * 512
            nf = min(512, Fdim - f0)
            nfc = nf // 128
            wg, Twg = rload(wg_d[:, f0:f0 + nf].rearrange("(k p) c -> p k c", p=128), 8, nf)
            wu, Twu = rload(wu_d[:, f0:f0 + nf].rearrange("(k p) c -> p k c", p=128), 8, nf)
            wd, Twd = rload(wd_d[f0:f0 + nf, :].rearrange("(j p) d -> p j d", p=128), nfc, D)
            def scale_wd(j):
                P.op("dve", lambda e: e.tensor_tensor(out=wd[:, j, :], in0=wd[:, j, :], in1=gate_bc[:], op=ALU.mult),
                     reads=[Twd, Tgate], writes=[Twd])
            for tt in range(4):
                ai = ffn_state["cnt"] % 2
                ffn_state["cnt"] += 1
                actb, Tact = A4[ai], TA4[ai]
                for fc in range(nfc):
                    c2 = ffn_state.setdefault("c2", 0)
                    ffn_state["c2"] = c2 + 1
                    g = gb[c2 % 2]; u = ub[c2 % 2]
                    sg, Tsg = F5[c2 % 2], TF5[c2 % 2]
                    for k in range(8):
                        P.op("pe", lambda e, g=g, k=k, fc=fc, wg=wg, tt=tt: e.matmul(banks[g][:], wg[:, k, fc * 128:(fc + 1) * 128], hT[:, k, tt * 512:(tt + 1) * 512],
                                                                                  start=(k == 0), stop=(k == 7)), reads=[Twg, Th[tt]], writes=[TB[g]])
                    for k in range(8):
                        P.op("pe", lambda e, u=u, k=k, fc=fc, wu=wu, tt=tt: e.matmul(banks[u][:], wu[:, k, fc * 128:(fc + 1) * 128], hT[:, k, tt * 512:(tt + 1) * 512],
                                                                                  start=(k == 0), stop=(k == 7)), reads=[Twu, Th[tt]], writes=[TB[u]])
                    P.op("act", lambda e, g=g, sg=sg: e.activation(out=sg[:], in_=banks[g][:], func=AF.Silu), reads=[TB[g], TB[u]], writes=[Tsg])
                    P.op("dve", lambda e, u=u, sg=sg, actb=actb, fc=fc: e.tensor_tensor(out=actb[:, fc, :], in0=sg[:], in1=banks[u][:], op=ALU.mult),
                         reads=[Tsg, TB[u]], writes=[Tact])
                    if tt == 0 and fc == nfc - 1:
                        scale_wd(0)
                    if tt == 1 and fc + 1 < nfc:
                        scale_wd(fc + 1)
                ffn_flush()
                ffn_state["pending"] = (actb, Tact, wd, Twd, nfc, tt, gate_col)

    def router(j):
        P.op("pool", lambda e: e.dma_start(out=wrs[:], in_=wr_d[j].rearrange("(k p) c -> p k c", p=128)), writes=[Twr], dma_out=Twr)
        b = nb()
        for i in range(NT):
            for k in range(8):
                P.op("pe", lambda e, i=i, k=k, b=b: e.matmul(banks[b][:, i * 8:(i + 1) * 8], hT[:, k, i * 128:(i + 1) * 128], wrs[:, k, :], start=(k == 0), stop=(k == 7)),
                     reads=[Th[i // 4], Twr], writes=[TB[b]])
        P.op("dve", lambda e, b=b: e.tensor_copy(out=lg[:], in_=banks[b][:, 0:128].rearrange("p (i c) -> p i c", c=8)), reads=[TB[b]], writes=[Tlg])
        m1 = m12[:, 0, :]; m2 = m12[:, 1, :]; den = m12[:, 2, :]
        bc = lambda a: a.unsqueeze(2).to_broadcast([128, NT, 8])
        P.op("dve", lambda e: e.tensor_reduce(out=m1, in_=lg[:], axis=AX.X, op=ALU.max), reads=[Tlg], writes=[Tlg])
        P.op("dve", lambda e: e.tensor_tensor(out=lg2, in0=lg[:], in1=bc(m1), op=ALU.is_equal), reads=[Tlg], writes=[Tlg, TE2])
        P.op("dve", lambda e: e.scalar_tensor_tensor(out=lg2, in0=lg2, scalar=-1e30, in1=lg[:], op0=ALU.mult, op1=ALU.add), reads=[Tlg, TE2], writes=[Tlg, TE2])
        P.op("dve", lambda e: e.tensor_reduce(out=m2, in_=lg2, axis=AX.X, op=ALU.max), reads=[Tlg, TE2], writes=[Tlg])
        P.op("dve", lambda e: e.tensor_tensor(out=lg2, in0=lg[:], in1=bc(m2), op=ALU.is_ge), reads=[Tlg], writes=[Tlg, TE2])
        P.op("dve", lambda e: e.tensor_tensor(out=lg[:], in0=lg[:], in1=bc(m1), op=ALU.subtract), reads=[Tlg], writes=[Tlg])
        P.op("act", lambda e: e.activation(out=lg[:], in_=lg[:], func=AF.Exp), reads=[Tlg], writes=[Tlg])
        P.op("dve", lambda e: e.tensor_tensor(out=lg[:], in0=lg[:], in1=lg2, op=ALU.mult), reads=[Tlg, TE2], writes=[Tlg])
        P.op("dve", lambda e: e.tensor_reduce(out=den, in_=lg[:], axis=AX.X, op=ALU.add), reads=[Tlg], writes=[Tlg])
        P.op("dve", lambda e: e.reciprocal(out=den, in_=den), reads=[Tlg], writes=[Tlg])
        P.op("dve", lambda e: e.tensor_tensor(out=gates[:], in0=lg[:], in1=bc(den), op=ALU.mult), reads=[Tlg], writes=[Tgates])

    gam = [1.0 - 2.0 ** (-5.0 - h) for h in range(4)]

    def mixer(l):
        win = win_d[l]
        qin, Tqin = A4[0], TA4[0]
        kT, TkT = A4[1], TA4[1]
        rv, Trv = A4[2], TA4[2]
        rg, Trg = A4[3], TA4[3]
        gv, Tgv = A4[4], TA4[4]
        gr, Tgr = A4[5], TA4[5]
        gq, Tgq = F1[0], TF1[0]
        gk, Tgk = F1[1], TF1[1]
        gq3 = gq[:].rearrange("p (a t) -> p a t", t=512)
        gk3 = gk[:].rearrange("p (a t) -> p a t", t=512)
        t1, Tt1 = F5[0], TF5[0]
        t2, Tt2 = F5[1], TF5[1]
        glv = F1[2][:].bitcast(BF16)
        PTgv = glv[:, 0:512]
        sqg = glv[:, 512:1024]
        kitmv = glv[:, 1024:1280]
        Tgl = TF1[2]
        P.op("sp", lambda e: e.dma_start(out=w2s[:], in_=w2_d[l]), writes=[Tw2], dma_out=Tw2)
        P.op("sp", lambda e: e.dma_start(out=b2s[:], in_=b2_d[l:l + 1, :]), writes=[Tw2], dma_out=Tw2)
        P.op("dve", lambda e: e.memset(Sr[:], 0.0), writes=[TSr])
        P.op("dve", lambda e: e.memset(Srb[:], 0.0), writes=[TSrb])
        P.op("dve", lambda e: e.memset(Sg[:], 0.0), writes=[TSg])

        def panel(c0, nc_):
            return rload(win[:, c0:c0 + nc_].rearrange("(k p) c -> p k c", p=128), 8, nc_)

        def fm_proj(pan, Tp, col, tt):
            b = nb()
            for k in range(8):
                P.op("pe", lambda e, b=b, k=k: e.matmul(banks[b][:], pan[:, k, col * 128:(col + 1) * 128], hT[:, k, tt * 512:(tt + 1) * 512], start=(k == 0), stop=(k == 7)),
                     reads=[Tp, Th[tt]], writes=[TB[b]])
            return b

        def tm_proj(pan, Tp, tt, dst, Tdst):
            for ti in range(4):
                b = nb()
                t0 = tt * 512 + ti * 128
                for k in range(8):
                    P.op("pe", lambda e, b=b, k=k, t0=t0: e.matmul(banks[b][:], hT[:, k, t0:t0 + 128], pan[:, k, :], start=(k == 0), stop=(k == 7)),
                         reads=[Tp, Th[tt]], writes=[TB[b]])
                P.op("act", lambda e, b=b, ti=ti: e.copy(out=dst[:, ti, :], in_=banks[b][:]), reads=[TB[b]], writes=[Tdst])

        for tt in range(4):
            tsl = slice(tt * 512, (tt + 1) * 512)
            for (c0, dst, Tdst, isq) in ((0, qin, Tqin, True), (1024, kT, TkT, False)):
                pa, Tpa = panel(c0, 512)
                pb, Tpb = panel(c0 + 512, 512)
                for h in range(4):
                    b1 = fm_proj(pa, Tpa, h, tt)
                    b2 = fm_proj(pb, Tpb, h, tt)
                    P.op("dve", lambda e, b1=b1: e.tensor_tensor(out=t1[:], in0=banks[b1][:], in1=cosT[:, tsl], op=ALU.mult), reads=[TB[b1], Ttab], writes=[Tt1])
                    P.op("dve", lambda e, b2=b2: e.tensor_tensor(out=t2[:], in0=banks[b2][:], in1=sinT[:, tsl], op=ALU.mult), reads=[TB[b2], Ttab], writes=[Tt2])
                    if isq:
                        P.op("dve", lambda e: e.tensor_tensor(out=t1[:], in0=t1[:], in1=t2[:], op=ALU.add), reads=[Tt1, Tt2], writes=[Tt1])
                        P.op("dve", lambda e, h=h: e.tensor_tensor(out=dst[:, h, :].rearrange("p (n c) -> p n c", c=128),
                                                                   in0=t1[:].rearrange("p (n c) -> p n c", c=128),
                                                                   in1=gqtab[:, h * 128:(h + 1) * 128].unsqueeze(1).to_broadcast([128, 4, 128]), op=ALU.mult),
                             reads=[Tt1, Ttab], writes=[Tdst])
                    else:
                        P.op("dve", lambda e, h=h: e.tensor_tensor(out=dst[:, h, :], in0=t1[:], in1=t2[:], op=ALU.add), reads=[Tt1, Tt2], writes=[Tdst])
            pa, Tpa = panel(2048, 512)
            tm_proj(pa, Tpa, tt, rv, Trv)
            pa, Tpa = panel(2560, 512)
            for h in range(4):
                b = fm_proj(pa, Tpa, h, tt)
                P.op("act", lambda e, b=b, h=h: e.activation(out=rg[:, h, :], in_=banks[b][:], func=AF.Silu), reads=[TB[b]], writes=[Trg])
            pa, Tpa = panel(3072, 512)
            for a in range(2):
                b = fm_proj(pa, Tpa, a, tt)
                P.op("act", lambda e, b=b, a=a: e.copy(out=gq3[:, a, :], in_=banks[b][:]), reads=[TB[b]], writes=[Tgq])
            for a in range(2):
                b = fm_proj(pa, Tpa, 2 + a, tt)
                P.op("act", lambda e, b=b, a=a: e.copy(out=gk3[:, a, :], in_=banks[b][:]), reads=[TB[b]], writes=[Tgk])
            pa, Tpa = panel(3584, 512)
            tm_proj(pa, Tpa, tt, gv, Tgv)
            pa, Tpa = panel(4096, 512)
            for h in range(4):
                b = fm_proj(pa, Tpa, h, tt)
                P.op("act", lambda e, b=b, h=h: e.activation(out=gr[:, h, :], in_=banks[b][:], func=AF.Silu), reads=[TB[b]], writes=[Tgr])
            if mix_stage < 1:
                continue
            pa, Tpa = panel(4608, 16)
            b = nb()
            for k in range(8):
                P.op("pe", lambda e, b=b, k=k: e.matmul(banks[b][0:16, :], pa[:, k, 0:16], hT[:, k, tsl], start=(k == 0), stop=(k == 7)),
                     reads=[Tpa, Th[tt]], writes=[TB[b]])
            P.op("act", lambda e, b=b: e.copy(out=gaT[0:16, :], in_=banks[b][0:16, :]), reads=[TB[b]], writes=[TgaT])

            def ret_chunk(n):
                csl = slice(n * 128, (n + 1) * 128)
                bS = 0
                for h in range(4):
                    P.op("pe", lambda e, h=h: e.matmul(banks[bS][:, h * 128:(h + 1) * 128], kT[:, h, csl], qin[:, h, csl], start=True, stop=True),
                         reads=[TkT, Tqin], writes=[TB[bS]])
                yield
                P.op("dve", lambda e: e.tensor_tensor(out=PTr[:], in0=banks[bS][:], in1=maskR[:], op=ALU.mult), reads=[TB[bS], Ttab], writes=[TPTr])
                yield
                bO = 1
                for h in range(4):
                    hs = slice(h * 128, (h + 1) * 128)
                    P.op("pe", lambda e, hs=hs: e.matmul(banks[bO][:, hs], rv[:, n, hs], PTr[:, hs], start=True, stop=False),
                         reads=[Trv, TPTr], writes=[TB[bO]])
                    P.op("pe", lambda e, hs=hs, h=h: e.matmul(banks[bO][:, hs], Srb[:, hs], qin[:, h, csl], start=False, stop=True),
                         reads=[TSrb, Tqin], writes=[TB[bO]])
                yield
                bK = 2
                bKv = banks[bK][:].bitcast(BF16)
                for h in range(4):
                    P.op("pe", lambda e, h=h: e.transpose(out=bKv[:, h * 128:(h + 1) * 128], in_=kT[:, h, csl], identity=idb[:]),
                         reads=[TkT, Tidb], writes=[TB[bK]])
                yield
                P.op("dve", lambda e: e.tensor_tensor(out=kst[:], in0=bKv[:, 0:512], in1=kdec[:], op=ALU.mult), reads=[TB[bK], Ttab], writes=[Tkst])
                yield
                bC = 3
                for h in range(4):
                    hs = slice(h * 128, (h + 1) * 128)
                    P.op("pe", lambda e, hs=hs: e.matmul(banks[bC][:, hs], kst[:, hs], rv[:, n, hs], start=True, stop=True),
                         reads=[Tkst, Trv], writes=[TB[bC]])
                yield
                for h in range(4):
                    hs = slice(h * 128, (h + 1) * 128)
                    P.op("dve", lambda e, hs=hs, h=h: e.scalar_tensor_tensor(out=Sr[:, hs], in0=Sr[:, hs], scalar=float(gam[h] ** 128), in1=banks[bC][:, hs], op0=ALU.mult, op1=ALU.add),
                         reads=[TSr, TB[bC]], writes=[TSr])
                yield
                P.op("act", lambda e: e.copy(out=Srb[:], in_=Sr[:]), reads=[TSr], writes=[TSrb])
                yield
                yield from head_norm(bO, 0, rg, Trg, 0, tt, n, F5[0], TF5[0], sq[:], Tsq)

            def gla_chunk(n):
                csl = slice(n * 128, (n + 1) * 128)
                bL = 4
                P.op("pe", lambda e: e.matmul(banks[bL][:, 0:256], gaT[0:16, csl], w2s[:], start=True, stop=False), reads=[TgaT, Tw2], writes=[TB[bL]])
                P.op("pe", lambda e: e.matmul(banks[bL][:, 0:256], ones_row[0:1, :], b2s[0:1, :], start=False, stop=True), reads=[Tones, Tw2], writes=[TB[bL]])
                yield
                P.op("act", lambda e: e.activation(out=esp[:], in_=banks[bL][:, 0:256], func=AF.Exp, scale=-1.0), reads=[TB[bL]], writes=[Tesp])
                yield
                P.op("act", lambda e: e.activation(out=esp[:], in_=esp[:], func=AF.Ln, bias=1.0), reads=[Tesp], writes=[Tesp])
                yield
                bB = 5
                for a in range(2):
                    P.op("pe", lambda e, a=a: e.matmul(banks[bB][:, a * 128:(a + 1) * 128], esp[:, a * 128:(a + 1) * 128], tri[:], start=True, stop=True),
                         reads=[Tesp, Tconst], writes=[TB[bB]])
                yield
                bT3 = banks[bB][:, 0:256].rearrange("p (a t) -> p a t", t=128)
                P.op("dve", lambda e: e.tensor_copy(out=sm[:, 0:2], in_=bT3[:, :, 63]), reads=[TB[bB]], writes=[Tsm])
                P.op("dve", lambda e: e.tensor_scalar(out=sm[:, 2:4], in0=bT3[:, :, 63], scalar1=-1.0, scalar2=None, op0=ALU.mult), reads=[TB[bB]], writes=[Tsm])
                P.op("dve", lambda e: e.tensor_copy(out=sm[:, 4:6], in_=bT3[:, :, 127]), reads=[TB[bB]], writes=[Tsm])
                yield
                P.op("dve", lambda e: e.tensor_tensor(out=sm[:, 12:14], in0=sm[:, 4:6], in1=sm[:, 0:2], op=ALU.subtract), reads=[Tsm], writes=[Tsm])
                yield
                for a in range(2):
                    asl = slice(a * 128, (a + 1) * 128)
                    P.op("act", lambda e, a=a, asl=asl: e.activation(out=E1[:, asl], in_=banks[bB][:, asl], func=AF.Exp, bias=sm[:, 2 + a:3 + a], scale=1.0),
                         reads=[TB[bB], Tsm], writes=[TE1])
                    P.op("act", lambda e, a=a, asl=asl: e.activation(out=E2[:, asl], in_=banks[bB][:, asl], func=AF.Exp, bias=sm[:, a:a + 1], scale=-1.0),
                         reads=[TB[bB], Tsm], writes=[TE2])
                yield
                P.op("act", lambda e: e.activation(out=sm[:, 6:8], in_=sm[:, 4:6], func=AF.Exp), reads=[Tsm], writes=[Tsm])
                P.op("act", lambda e: e.activation(out=sm[:, 8:10], in_=sm[:, 12:14], func=AF.Exp), reads=[Tsm], writes=[Tsm])
                P.op("act", lambda e: e.activation(out=sm[:, 10:12], in_=sm[:, 0:2], func=AF.Exp), reads=[Tsm], writes=[Tsm])
                yield
                for X in range(2):
                    P.op("dve", lambda e, X=X: e.scalar_tensor_tensor(out=qi2[:, X, :, :], in0=gq3[:, :, csl], scalar=rmask[:, X:X + 1],
                                                                      in1=E1[:].rearrange("p (a t) -> p a t", t=128), op0=ALU.mult, op1=ALU.mult),
                         reads=[Tgq, TE1, Tconst], writes=[Tqi])
                P.op("dve", lambda e: e.tensor_tensor(out=ki[:], in0=gk3[:, :, csl], in1=E2[:].rearrange("p (a t) -> p a t", t=128), op=ALU.mult),
                     reads=[Tgk, TE2], writes=[Tki])
                yield
                bS = 4
                for h in range(4):
                    a = h // 2
                    P.op("pe", lambda e, h=h, a=a: e.matmul(banks[bS][:, h * 128:(h + 1) * 128], ki[:, a, :], qi2[:, h % 2, a, :], start=True, stop=True),
                         reads=[Tki, Tqi], writes=[TB[bS]])
                yield
                P.op("dve", lambda e: e.tensor_tensor(out=PTgv, in0=banks[bS][:], in1=maskG[:], op=ALU.mult), reads=[TB[bS], Ttab], writes=[Tgl])
                for a in range(2):
                    P.op("dve", lambda e, a=a: e.tensor_scalar(out=Sgb[:, a, :], in0=Sg[:, a, :], scalar1=sm[:, 10 + a:11 + a], scalar2=0.125, op0=ALU.mult, op1=ALU.mult),
                         reads=[TSg, Tsm], writes=[TSgb])
                yield
                bO = 6
                for h in range(4):
                    a = h // 2
                    hs = slice(h * 128, (h + 1) * 128)
                    P.op("pe", lambda e, hs=hs: e.matmul(banks[bO][:, hs], gv[:, n, hs], PTgv[:, hs], start=True, stop=False),
                         reads=[Tgv, Tgl], writes=[TB[bO]])
                    P.op("pe", lambda e, hs=hs, a=a, h=h: e.matmul(banks[bO][:, hs], Sgb[:, a, :], qi2[:, h % 2, a, :], start=False, stop=True),
                         reads=[TSgb, Tqi], writes=[TB[bO]])
                yield
                bK = 7
                bKv = banks[bK][:].bitcast(BF16)
                for a in range(2):
                    P.op("pe", lambda e, a=a: e.transpose(out=bKv[:, a * 128:(a + 1) * 128], in_=ki[:, a, :], identity=idb[:]),
                         reads=[Tki, Tidb], writes=[TB[bK]])
                yield
                P.op("act", lambda e: e.copy(out=kitmv, in_=bKv[:, 0:256]), reads=[TB[bK]], writes=[Tgl])
                yield
                bC = 5
                for a in range(2):
                    P.op("pe", lambda e, a=a: e.matmul(banks[bC][:, a * 256:(a + 1) * 256], kitmv[:, a * 128:(a + 1) * 128], gv[:, n, a * 256:(a + 1) * 256], start=True, stop=True),
                         reads=[Tgl, Tgv], writes=[TB[bC]])
                yield
                for a in range(2):
                    for hh in range(2):
                        ps_ = slice(64 * hh, 64 * hh + 64)
                        cs0 = a * 256 + hh * 128
                        P.op("dve", lambda e, a=a, ps_=ps_, cs0=cs0: e.tensor_scalar(out=tmpc[ps_, a, :], in0=banks[bC][ps_, cs0:cs0 + 128], scalar1=sm[ps_, 8 + a:9 + a], scalar2=None, op0=ALU.mult),
                             reads=[TB[bC], Tsm], writes=[Ttmpc])
                yield
                for a in range(2):
                    P.op("dve", lambda e, a=a: e.scalar_tensor_tensor(out=Sg[:, a, :], in0=Sg[:, a, :], scalar=sm[:, 6 + a:7 + a], in1=tmpc[:, a, :], op0=ALU.mult, op1=ALU.add),
                         reads=[TSg, Tsm, Ttmpc], writes=[TSg])
                yield
                yield from head_norm(bO, 4, gr, Tgr, 4, tt, n, F5[2], TF5[2], sqg, Tgl)

            if mix_stage >= 2:
                for n in range(4):
                    active = [ret_chunk(n), gla_chunk(n)]
                    while active:
                        for g in list(active):
                            try:
                                next(g)
                            except StopIteration:
                                active.remove(g)

            if mix_stage < 8:
                continue
            for dt in range(2):
                wo, Two = rload(wout_d[l][:, dt * 512:(dt + 1) * 512].rearrange("(k p) c -> p k c", p=128), 8, 512)
                for k in range(8):
                    P.op("dve", lambda e, wo=wo, k=k, dt=dt: e.tensor_tensor(out=wo[:, k, :], in0=wo[:, k, :], in1=gate_bc[:, dt * 512:(dt + 1) * 512], op=ALU.mult),
                         reads=[Two, Tgate], writes=[Two])
                for ti in range(4):
                    tile = tt * 4 + ti
                    b = nb()
                    t0 = tt * 512 + ti * 128
                    for k in range(8):
                        P.op("pe", lambda e, b=b, k=k, t0=t0, wo=wo: e.matmul(banks[b][:], hT[:, k, t0:t0 + 128], wo[:, k, :], start=(k == 0), stop=(k == 7)),
                             reads=[Th[tt], Two], writes=[TB[b]])
                    xs = xt[:, tile, dt * 512:(dt + 1) * 512]
                    P.op("dve", lambda e, b=b, xs=xs: e.tensor_tensor(out=xs, in0=banks[b][:], in1=xs, op=ALU.add), reads=[TB[b], Tx[tile]], writes=[Tx[tile]])

    def head_norm(bO, bs, gT, TgT, k0, tt, n, Abuf, TA, sqb, Tsqb):
        P.op("act", lambda e: e.activation(out=sqb, in_=banks[bO][:], func=AF.Square), reads=[TB[bO]], writes=[Tsqb])
        yield
        P.op("pe", lambda e: e.matmul(banks[bs][:], onesb[:], sqb, start=True, stop=True), reads=[Tones, Tsqb], writes=[TB[bs]])
        yield
        P.op("act", lambda e: e.activation(out=Abuf[:], in_=banks[bs][:], func=AF.Ln, bias=EPS, scale=1.0), reads=[TB[bs]], writes=[TA])
        yield
        P.op("act", lambda e: e.activation(out=Abuf[:], in_=Abuf[:], func=AF.Exp, scale=-0.5), reads=[TA], writes=[TA])
        yield
        P.op("dve", lambda e: e.tensor_tensor(out=Abuf[:], in0=banks[bO][:], in1=Abuf[:], op=ALU.mult), reads=[TB[bO], TA], writes=[TA])
        yield
        t0 = tt * 512 + n * 128
        P.op("dve", lambda e: e.tensor_tensor(out=hT[:, k0:k0 + 4, t0:t0 + 128], in0=Abuf[:].rearrange("p (h c) -> p h c", c=128),
                                              in1=gT[:, :, n * 128:(n + 1) * 128], op=ALU.mult), reads=[TA, TgT], writes=[Th[tt]])
        yield

    for l in (layers if layers is not None else range(depth)):
        if do_mixer:
            norm_to_hT(l, 0)
            mixer(l)
        if do_ffn:
            norm_to_hT(l, 1)
            j = l // 2
            if l % 2 == 0:
                ffn(dwg_d[j], dwu_d[j], dwd_d[j], 2816, None)
                ffn_flush()
            else:
                router(j)
                for ex in range(8):
                    ffn(mwg_d[j, ex], mwu_d[j, ex], mwd_d[j, ex], 3584, ex)
                ffn_flush()

    gfb, Tgfb = F1[0], TF1[0]
    P.op("sp", lambda e: e.dma_start(out=gfb[:], in_=gfin_d.to_broadcast([128, D])), writes=[Tgfb], dma_out=Tgfb)
    rms_stats()
    for i in range(NT):
        ob, Tob = F1[1 + i % 2], TF1[1 + i % 2]
        P.op("dve", lambda e, i=i, ob=ob: e.scalar_tensor_tensor(out=ob[:], in0=xt[:, i, :], scalar=rstd[:, i:i + 1], in1=gfb[:], op0=ALU.mult, op1=ALU.mult),
             reads=[Tx[i], Trstd, Tgfb], writes=[Tob])
        P.op("sp", lambda e, i=i, ob=ob: e.dma_start(out=out_d[i * 128:(i + 1) * 128, :], in_=ob[:]), reads=[Tob], writes=[Tout], dma_out=Tout)
    P.op("sp", lambda e: e.nop(), reads=[Tout] + TF1)
    P.run()
    return nc


def _consts():
    f = np.float32
    idf = np.eye(128, dtype=f)
    s = np.arange(128)
    tri = np.where(s[:, None] <= s[None, :], -1.0 / 16.0, 0.0).astype(f)
    d = np.arange(128)
    j = d % 64
    inv = (10000.0 ** (-(j.astype(np.float64)) / 64.0))
    t = np.arange(S, dtype=np.float64)
    ang = inv[:, None] * t[None, :]
    cos = np.cos(ang).astype(f)
    sin = np.sin(ang)
    sin = np.where(d[:, None] < 64, -sin, sin).astype(f)
    gam = np.array([1.0 - 2.0 ** (-5.0 - h) for h in range(4)], dtype=np.float64)
    maskR = np.zeros((128, 512), f)
    kdec = np.zeros((128, 512), f)
    gqtab = np.zeros((128, 512), f)
    maskG = np.zeros((128, 512), f)
    causal = (s[:, None] <= s[None, :])
    for h in range(4):
        maskR[:, h * 128:(h + 1) * 128] = np.where(causal, (gam[h] ** (-(s[:, None] + 1.0))) * (128.0 ** -0.5), 0.0)
        kdec[:, h * 128:(h + 1) * 128] = ((gam[h] ** (127.0 - s)) * (128.0 ** -0.5))[:, None]
        gqtab[:, h * 128:(h + 1) * 128] = (gam[h] ** (s + 1.0))[None, :]
        maskG[:, h * 128:(h + 1) * 128] = np.where(causal, 64.0 ** -0.5, 0.0)
    rmask = np.zeros((128, 2), f)
    rmask[:64, 0] = 1.0
    rmask[64:, 1] = 1.0
    return dict(idf=idf, tri=tri, cos=cos, sin=sin, maskR=maskR, kdec=kdec, gqtab=gqtab, maskG=maskG, rmask=rmask)


def _win_cols():
    sw = np.concatenate([h * 128 + (np.arange(128) + 64) % 128 for h in range(4)])
    rq = np.arange(0, 512); rk = np.arange(512, 1024); rv = np.arange(1024, 1536); rg = np.arange(1536, 2048)
    gq = np.arange(2048, 2304); gk = np.arange(2304, 2560); gv = np.arange(2560, 3072); gr = np.arange(3072, 3584)
    ga = np.arange(3584, 3600)
    return np.concatenate([rq, rq[sw], rk, rk[sw], rv, rg, gq, gk, gv, gr, ga])


def make_in_maps(inputs, n_cores=8):
    f = np.float32
    g = lambda k: np.ascontiguousarray(np.asarray(inputs[k], dtype=f))
    shared = dict(
        ada_w=g("ada_w"), ada_b=g("ada_b"), gmix=g("norm_mix_g"), gffn=g("norm_ffn_g"),
        gfin=g("final_g").reshape(1, D),
        w_in=np.ascontiguousarray(g("w_in")[:, :, _win_cols()]),
        w2=g("w_gla_gate2"), b2=g("b_gla_gate"), w_out=g("w_out"),
        dwg=g("dense_w_gate"), dwu=g("dense_w_up"), dwd=g("dense_w_down"),
        wr=g("w_router"), mwg=g("moe_w_gate"), mwu=g("moe_w_up"), mwd=g("moe_w_down"),
    )
    shared.update(_consts())
    x = g("x")
    c = g("c")
    maps = []
    for b in range(n_cores):
        m = dict(shared)
        m["x"] = np.ascontiguousarray(x[b])
        m["c"] = np.ascontiguousarray(c[b].reshape(8, 128).T)
        maps.append(m)
    return maps


_NC = None


def kernel(**inputs):
    global _NC
    if _NC is None:
        _NC = build()
    maps = make_in_maps(inputs, 8)
    res = run_bass_kernel_spmd(_NC, maps, core_ids=list(range(8)))
    return np.stack([np.asarray(r["out"], dtype=np.float32) for r in res.results], axis=0)
```
